# Optimizing a Trainium2 kernel written in Bass

```python
import jax, jax.numpy as jnp
from jax import lax
import numpy as np

D_MODEL = 2048
BATCH = 1
SEQ = 16384
DEPTH = 2

SB_HEAD_DIM = 128
SB_HEADS = D_MODEL // (2 * SB_HEAD_DIM)
SB_WIDTH = SB_HEADS * SB_HEAD_DIM
Q_BLOCK = 128
K_CHUNK = 128
SSM_WIDTH = D_MODEL // 2
SSM_GROUP = 16
SSM_GROUPS = SSM_WIDTH // SSM_GROUP
SSM_STATE = 64
DT_MIN = 0.001
DT_MAX = 0.1
IN_COLS = 3 * SB_WIDTH + SSM_WIDTH + 2 * D_MODEL
D_FF_DENSE = 2 * D_MODEL
N_EXPERTS = 8
TOP_K = 2
D_FF_EXPERT = D_MODEL // 2
N_DENSE = (DEPTH + 1) // 2
N_MOE = DEPTH // 2
RMS_EPS = 1e-6

kernel_name = 'hybrid_stickbreak_s5_moe_trunk'


def rms_norm(x, g):
    x32 = x.astype(jnp.float32)
    y = x32 * lax.rsqrt(jnp.mean(x32 * x32, axis=-1, keepdims=True) + RMS_EPS)
    return (y * g.astype(jnp.float32)).astype(x.dtype)


def suffix_sum(m, n_chunks):
    mc = m.reshape(m.shape[:-1] + (n_chunks, K_CHUNK))
    ci = jnp.arange(K_CHUNK)
    tri = (ci[:, None] >= ci[None, :]).astype(m.dtype)
    ni = jnp.arange(n_chunks)
    strict = (ni[:, None] > ni[None, :]).astype(m.dtype)
    within = jnp.einsum('bhqnc,cd->bhqnd', mc, tri)
    after = jnp.einsum('bhqn,np->bhqp', jnp.sum(mc, axis=-1), strict)
    return (within + after[..., None]).reshape(m.shape)


def stick_breaking_attention(q, k, v):
    b, s, h, dh = q.shape
    nb = s // Q_BLOCK
    scale = dh ** -0.5
    qh = q.transpose(0, 2, 1, 3)
    kh = k.transpose(0, 2, 1, 3)
    vh = v.transpose(0, 2, 1, 3)
    outs = []
    for blk in range(nb):
        lk = (blk + 1) * Q_BLOCK
        q_blk = qh[:, :, blk * Q_BLOCK:lk]
        z = jnp.einsum('bhqd,bhkd->bhqk', q_blk, kh[:, :, :lk],
                       preferred_element_type=jnp.float32) * scale
        q_pos = blk * Q_BLOCK + jnp.arange(Q_BLOCK)
        causal = jnp.arange(lk)[None, :] < q_pos[:, None]
        sp = jnp.where(causal, jax.nn.softplus(z), 0.0)
        r = suffix_sum(sp, lk // K_CHUNK)
        w = jnp.exp(jnp.where(causal, z - r, -jnp.inf))
        outs.append(jnp.einsum('bhqk,bhkd->bhqd', w.astype(vh.dtype), vh[:, :, :lk]))
    out = jnp.concatenate(outs, axis=2)
    return out.transpose(0, 2, 1, 3).reshape(b, s, h * dh)


def s5_ssm(u, a_re, a_im, log_dt, b_re, b_im, c_re, c_im, d_skip):
    bsz, s, _ = u.shape
    f32 = jnp.float32
    u32 = u.astype(f32).reshape(bsz, s, SSM_GROUPS, SSM_GROUP)
    dt = jnp.exp(log_dt.astype(f32))[:, None]
    ar = a_re.astype(f32)
    ai = a_im.astype(f32)
    mag = jnp.exp(dt * ar)
    abar_re = mag * jnp.cos(dt * ai)
    abar_im = mag * jnp.sin(dt * ai)
    den = ar * ar + ai * ai
    num_re = abar_re - 1.0
    zoh_re = (num_re * ar + abar_im * ai) / den
    zoh_im = (abar_im * ar - num_re * ai) / den
    br = b_re.astype(f32)
    bi = b_im.astype(f32)
    bbar_re = zoh_re[..., None] * br - zoh_im[..., None] * bi
    bbar_im = zoh_re[..., None] * bi + zoh_im[..., None] * br
    bu_re = jnp.einsum('bsgp,gnp->bsgn', u32, bbar_re)
    bu_im = jnp.einsum('bsgp,gnp->bsgn', u32, bbar_im)
    a_re_t = jnp.broadcast_to(abar_re, bu_re.shape)
    a_im_t = jnp.broadcast_to(abar_im, bu_im.shape)

    def combine(left, right):
        l_ar, l_ai, l_br, l_bi = left
        r_ar, r_ai, r_br, r_bi = right
        return (r_ar * l_ar - r_ai * l_ai,
                r_ar * l_ai + r_ai * l_ar,
                r_ar * l_br - r_ai * l_bi + r_br,
                r_ar * l_bi + r_ai * l_br + r_bi)

    _, _, x_re, x_im = lax.associative_scan(combine, (a_re_t, a_im_t, bu_re, bu_im), axis=1)
    y = (jnp.einsum('bsgn,gpn->bsgp', x_re, c_re.astype(f32))
         - jnp.einsum('bsgn,gpn->bsgp', x_im, c_im.astype(f32))
         + d_skip.astype(f32) * u32)
    return y.reshape(bsz, s, SSM_WIDTH).astype(u.dtype)


def hybrid_mixer(h, w_in, a_re, a_im, log_dt, b_re, b_im, c_re, c_im, d_skip,
                 w_glu, p_attn, p_ssm, w_out):
    b, s, _ = h.shape
    proj = h @ w_in
    cuts = [SB_WIDTH, 2 * SB_WIDTH, 3 * SB_WIDTH, 3 * SB_WIDTH + SSM_WIDTH,
            3 * SB_WIDTH + SSM_WIDTH + D_MODEL]
    q, k, v, u, gate_a, gate_b = jnp.split(proj, cuts, axis=-1)
    heads = lambda t: t.reshape(b, s, SB_HEADS, SB_HEAD_DIM)
    o_attn = stick_breaking_attention(heads(q), heads(k), heads(v))
    y = jax.nn.gelu(s5_ssm(u, a_re, a_im, log_dt, b_re, b_im, c_re, c_im, d_skip))
    o_ssm = y * jax.nn.sigmoid(y @ w_glu)
    merged = (jax.nn.sigmoid(gate_a) * (o_attn @ p_attn)
              + jax.nn.sigmoid(gate_b) * (o_ssm @ p_ssm))
    return merged @ w_out


def swiglu(h, w_gate, w_up, w_down):
    return (jax.nn.silu(h @ w_gate) * (h @ w_up)) @ w_down


def moe_swiglu(h, w_router, w_gate, w_up, w_down):
    logits = jnp.einsum('bsd,de->bse', h, w_router, preferred_element_type=jnp.float32)
    top_val, top_idx = lax.top_k(logits, TOP_K)
    gates = jax.nn.softmax(top_val, axis=-1)
    weights = jnp.sum(jax.nn.one_hot(top_idx, N_EXPERTS, dtype=jnp.float32)
                      * gates[..., None], axis=-2)
    out = jnp.zeros_like(h)
    for e in range(N_EXPERTS):
        out = out + weights[..., e:e + 1].astype(h.dtype) * swiglu(h, w_gate[e], w_up[e], w_down[e])
    return out


def setup_inputs(seed: int = 0) -> dict:
    key = jax.random.key(seed)
    ks = jax.random.split(key, 26)
    f32 = jnp.float32
    nrm = lambda k, shape, scale: jax.random.normal(k, shape, f32) * scale
    n_idx = jnp.arange(SSM_STATE, dtype=f32)
    a_re = -0.5 + nrm(ks[5], (DEPTH, SSM_GROUPS, SSM_STATE), 0.01)
    a_im = jnp.pi * n_idx + nrm(ks[6], (DEPTH, SSM_GROUPS, SSM_STATE), 0.01)
    log_dt = jax.random.uniform(ks[7], (DEPTH, SSM_GROUPS), f32,
                                float(np.log(DT_MIN)), float(np.log(DT_MAX)))
    b_scale = (2.0 * SSM_GROUP) ** -0.5
    c_scale = (2.0 * SSM_STATE) ** -0.5
    return {
        'x': nrm(ks[0], (BATCH, SEQ, D_MODEL), 1.0),
        'mix_norm': 1.0 + nrm(ks[1], (DEPTH, D_MODEL), 0.02),
        'ffn_norm': 1.0 + nrm(ks[2], (DEPTH, D_MODEL), 0.02),
        'final_norm': 1.0 + nrm(ks[3], (D_MODEL,), 0.02),
        'w_in': nrm(ks[4], (DEPTH, D_MODEL, IN_COLS), D_MODEL ** -0.5),
        'ssm_a_re': a_re,
        'ssm_a_im': a_im,
        'ssm_log_dt': log_dt,
        'ssm_b_re': nrm(ks[8], (DEPTH, SSM_GROUPS, SSM_STATE, SSM_GROUP), b_scale),
        'ssm_b_im': nrm(ks[9], (DEPTH, SSM_GROUPS, SSM_STATE, SSM_GROUP), b_scale),
        'ssm_c_re': nrm(ks[10], (DEPTH, SSM_GROUPS, SSM_GROUP, SSM_STATE), c_scale),
        'ssm_c_im': nrm(ks[11], (DEPTH, SSM_GROUPS, SSM_GROUP, SSM_STATE), c_scale),
        'ssm_d': nrm(ks[12], (DEPTH, SSM_GROUPS, SSM_GROUP), 1.0),
        'w_glu': nrm(ks[13], (DEPTH, SSM_WIDTH, SSM_WIDTH), SSM_WIDTH ** -0.5),
        'p_attn': nrm(ks[14], (DEPTH, SB_WIDTH, D_MODEL), SB_WIDTH ** -0.5),
        'p_ssm': nrm(ks[15], (DEPTH, SSM_WIDTH, D_MODEL), SSM_WIDTH ** -0.5),
        'w_out': nrm(ks[16], (DEPTH, D_MODEL, D_MODEL), D_MODEL ** -0.5),
        'ffn_w_gate': nrm(ks[17], (N_DENSE, D_MODEL, D_FF_DENSE), D_MODEL ** -0.5),
        'ffn_w_up': nrm(ks[18], (N_DENSE, D_MODEL, D_FF_DENSE), D_MODEL ** -0.5),
        'ffn_w_down': nrm(ks[19], (N_DENSE, D_FF_DENSE, D_MODEL), D_FF_DENSE ** -0.5),
        'w_router': nrm(ks[20], (N_MOE, D_MODEL, N_EXPERTS), D_MODEL ** -0.5),
        'moe_w_gate': nrm(ks[21], (N_MOE, N_EXPERTS, D_MODEL, D_FF_EXPERT), D_MODEL ** -0.5),
        'moe_w_up': nrm(ks[22], (N_MOE, N_EXPERTS, D_MODEL, D_FF_EXPERT), D_MODEL ** -0.5),
        'moe_w_down': nrm(ks[23], (N_MOE, N_EXPERTS, D_FF_EXPERT, D_MODEL), D_FF_EXPERT ** -0.5),
    }


def reference(x, mix_norm, ffn_norm, final_norm, w_in, ssm_a_re, ssm_a_im, ssm_log_dt,
              ssm_b_re, ssm_b_im, ssm_c_re, ssm_c_im, ssm_d, w_glu, p_attn, p_ssm, w_out,
              ffn_w_gate, ffn_w_up, ffn_w_down, w_router, moe_w_gate, moe_w_up, moe_w_down):
    for layer in range(DEPTH):
        h = rms_norm(x, mix_norm[layer])
        x = x + hybrid_mixer(h, w_in[layer], ssm_a_re[layer], ssm_a_im[layer], ssm_log_dt[layer],
                             ssm_b_re[layer], ssm_b_im[layer], ssm_c_re[layer], ssm_c_im[layer],
                             ssm_d[layer], w_glu[layer], p_attn[layer], p_ssm[layer], w_out[layer])
        h = rms_norm(x, ffn_norm[layer])
        i = layer // 2
        if layer % 2 == 0:
            x = x + swiglu(h, ffn_w_gate[i], ffn_w_up[i], ffn_w_down[i])
        else:
            x = x + moe_swiglu(h, w_router[i], moe_w_gate[i], moe_w_up[i], moe_w_down[i])
    return rms_norm(x, final_norm)
```

```python
import numpy as np
import ml_dtypes
import concourse.bass as bass
import concourse.mybir as mybir
from concourse.bass_utils import run_bass_kernel_spmd

F32 = mybir.dt.float32
BF16 = mybir.dt.bfloat16
AF = mybir.ActivationFunctionType
ALU = mybir.AluOpType
NPBF = ml_dtypes.bfloat16


class Sem:
    def __init__(self, nc, name, unit):
        self.h = nc.semaphore(name).__enter__()
        self.unit = unit
        self.count = 0


class Buf:
    __slots__ = ("name", "lw", "rd")

    def __init__(self, name):
        self.name = name
        self.lw = None
        self.rd = {}


class Stream:
    def __init__(self, ctx, name, sync_self):
        self.ctx = ctx
        self.name = name
        self.csem = Sem(ctx.nc, "c_" + name, 1)
        self.sync_self = sync_self
        self.known = {}
        self.items = []

    def _deps(self, reads, writes, ignore=None):
        deps = {}
        for b in reads:
            if b.lw is not None:
                s, v = b.lw
                if deps.get(s, 0) < v:
                    deps[s] = v
        for b in writes:
            if b.lw is not None:
                s, v = b.lw
                if deps.get(s, 0) < v:
                    deps[s] = v
            for s, v in b.rd.items():
                if deps.get(s, 0) < v:
                    deps[s] = v
        waits = []
        for s, v in deps.items():
            if s is self.csem and not self.sync_self:
                continue
            if ignore is not None and s is ignore[0] and v > ignore[1]:
                continue
            if self.known.get(s, 0) < v:
                waits.append((s, v))
                self.known[s] = v
        return waits

    def _mark(self, ev, reads, writes):
        s, v = ev
        for b in reads:
            if b.rd.get(s, 0) < v:
                b.rd[s] = v
        for b in writes:
            b.lw = ev
            b.rd = {}

    def op(self, fn, reads=(), writes=(), last=True):
        waits = self._deps(reads, writes)
        ev = (self.csem, self.csem.count + 1)
        if last:
            self.csem.count += 1
        self.items.append((waits, fn, ev if last else None))
        self._mark(ev, reads, writes)

    def dma(self, fn, sem, reads=(), writes=(), final=None):
        waits = self._deps(reads, writes, ignore=(sem, sem.count) if final is not None else None)
        sem.count += 1
        ev = (sem, sem.count)
        self.items.append((waits, fn, ev))
        self._mark((sem, final if final is not None else sem.count), reads, writes)

    def wait_for(self, sem):
        if sem.count > 0 and self.known.get(sem, 0) < sem.count:
            self.items.append(([(sem, sem.count)], None, None))
            self.known[sem] = sem.count

    def emit(self, eng):
        for waits, fn, ev in self.items:
            for s, v in waits:
                eng.wait_ge(s.h, v * s.unit)
            if fn is None:
                continue
            ins = fn(eng)
            if ev is not None:
                ins.then_inc(ev[0].h, ev[0].unit)
        self.items = []


class Ctx:
    def __init__(self):
        self.nc = bass.Bass("TRN2", target_bir_lowering=False)
        nc = self.nc
        self.pe = Stream(self, "pe", False)
        self.act = Stream(self, "act", True)
        self.dve = Stream(self, "dve", True)
        self.pool = Stream(self, "pool", True)
        self.sp = Stream(self, "sp", True)
        self.dsems = []
        self._n = 0

    def dsem(self, name=None):
        self._n += 1
        s = Sem(self.nc, name or f"d{self._n}", 16)
        self.dsems.append(s)
        return s

    def sb(self, name, shape, dt):
        return self.nc.sbuf_tensor(name, list(shape), dt).__enter__()

    def ps(self, name, shape, dt=F32):
        return self.nc.psum_tensor(name, list(shape), dt).__enter__()

    def dram(self, name, shape, dt, kind):
        return self.nc.dram_tensor(name, list(shape), dt, kind=kind).ap()

    def finish(self):
        for s in self.dsems:
            self.sp.wait_for(s)
        nc = self.nc
        with nc.Block() as block:
            block.sync(lambda e: self.sp.emit(e))
            block.tensor(lambda e: self.pe.emit(e))
            block.scalar(lambda e: self.act.emit(e))
            block.vector(lambda e: self.dve.emit(e))
            block.gpsimd(lambda e: self.pool.emit(e))
        return nc

D = 2048
KD = 16
TP = 1024
NTB = TP // 512
EPS = 1e-6


class TokCommon:
    def __init__(self, ctx, NW=4, NB=3, split_cast=False):
        self.ctx = ctx
        self.split_cast = split_cast
        c = ctx
        self.pt = [c.ps(f"pt{i}", [128, 1024]) for i in range(4)]
        self.ptB = [Buf(f"pt{i}") for i in range(4)]
        self.pti = 0
        self.NW, self.NB = NW, NB
        self.wst = [c.sb(f"wst{i}", [128, 2048], F32) for i in range(self.NW)]
        self.wstB = [Buf(f"wst{i}") for i in range(self.NW)]
        self.wstS = [c.dsem(f"wst{i}") for i in range(self.NW)]
        self.wbf = [c.sb(f"wbf{i}", [128, 2048], BF16) for i in range(self.NB)]
        self.wbfB = [Buf(f"wbf{i}") for i in range(self.NB)]
        self.wi = 0
        self.ones = c.sb("ones", [128, 128], BF16)
        self.onesB = Buf("ones")
        c.pool.op(lambda e: e.memset(self.ones[:], 1.0), writes=[self.onesB])
        self.sq = [c.sb(f"sq{i}", [128, TP], BF16) for i in range(2)]
        self.sqB = [Buf(f"sq{i}") for i in range(2)]
        self.rstd = c.sb("rstd", [128, TP], F32)
        self.rstdB = Buf("rstd")

    def next_pt(self):
        i = self.pti % 4
        self.pti += 1
        return self.pt[i], self.ptB[i]

    def norm(self, xT, xTB, gcol, gcolB, gidx, hT, hTB):
        c = self.ctx
        ps, psB = self.next_pt()
        for m in range(KD):
            s = m % 2
            c.act.op(lambda e, m=m, s=s: e.activation(out=self.sq[s][:], in_=xT[:, m, :], func=AF.Square),
                     reads=[xTB[m]], writes=[self.sqB[s]])
            for tb in range(NTB):
                c.pe.op(lambda e, m=m, s=s, tb=tb: e.matmul(ps[:, tb * 512:(tb + 1) * 512], self.ones[:],
                                                            self.sq[s][:, tb * 512:(tb + 1) * 512],
                                                            start=(m == 0), stop=(m == KD - 1)),
                        reads=[self.onesB, self.sqB[s]], writes=[psB], last=(tb == NTB - 1))
        c.dve.op(lambda e: e.tensor_scalar(out=self.rstd[:], in0=ps[:], scalar1=1.0 / D, scalar2=EPS,
                                           op0=ALU.mult, op1=ALU.add),
                 reads=[psB], writes=[self.rstdB])
        c.act.op(lambda e: e.activation(out=self.rstd[:], in_=self.rstd[:], func=AF.Ln),
                 reads=[self.rstdB], writes=[self.rstdB])
        c.act.op(lambda e: e.activation(out=self.rstd[:], in_=self.rstd[:], func=AF.Exp, scale=-0.5),
                 reads=[self.rstdB], writes=[self.rstdB])
        if hT is None:
            return
        for m in range(KD):
            c.dve.op(lambda e, m=m: e.scalar_tensor_tensor(out=hT[:, m, :], in0=xT[:, m, :],
                                                           scalar=gcol[:, gidx * KD + m:gidx * KD + m + 1],
                                                           in1=self.rstd[:], op0=ALU.mult, op1=ALU.mult),
                     reads=[xTB[m], gcolB, self.rstdB], writes=[hTB[m]])

    def norm_stats(self, xT, xTB):
        self.norm(xT, xTB, None, None, 0, None, None)

    def linear(self, Wt, KC, J, rhs_fn, evac_fn, j0=0):
        c = self.ctx
        for j in range(j0, j0 + J):
            a = self.wi % self.NW
            b = self.wi % self.NB
            self.wi += 1
            c.sp.dma(lambda e, j=j, a=a: e.dma_start(out=self.wst[a][:, :KC * 128], in_=Wt[j]),
                     self.wstS[a], writes=[self.wstB[a]])
            ceng = c.dve if (self.split_cast and self.wi % 2 == 1) else c.pool
            ceng.op(lambda e, a=a, b=b: e.tensor_copy(out=self.wbf[b][:, :KC * 128], in_=self.wst[a][:, :KC * 128]),
                    reads=[self.wstB[a]], writes=[self.wbfB[b]])
            ps, psB = self.next_pt()
            for kc in range(KC):
                for tb in range(NTB):
                    rap, rbufs = rhs_fn(kc, tb)
                    c.pe.op(lambda e, b=b, kc=kc, tb=tb, rap=rap, ps=ps: e.matmul(
                        ps[:, tb * 512:(tb + 1) * 512], self.wbf[b][:, kc * 128:(kc + 1) * 128], rap,
                        start=(kc == 0), stop=(kc == KC - 1)),
                        reads=[self.wbfB[b]] + rbufs, writes=[psB], last=(kc == KC - 1 and tb == NTB - 1))
            evac_fn(j, ps, psB)


def wtiles(W):
    K, N = W.shape
    return np.ascontiguousarray(W.reshape(K // 128, 128, N // 128, 128).transpose(2, 1, 0, 3).reshape(N // 128, 128, (K // 128) * 128))


def gcols(gs):
    return np.ascontiguousarray(np.concatenate([g.reshape(16, 128).T for g in gs], axis=1)).astype(np.float32)


def to_xT(xc):
    T = xc.shape[0]
    return np.ascontiguousarray(xc.T.reshape(16, 128, T))


def build_phaseA(ntok, ncol_tiles=32, dbg=False):
    ctx = Ctx()
    c = ctx
    xT_d = c.dram("xT", [16, 128, ntok], F32, "ExternalInput")
    g_d = c.dram("gcol", [128, 16], F32, "ExternalInput")
    w_d = c.dram("w_qkvu", [ncol_tiles, 128, KD * 128], F32, "ExternalInput")
    out_d = c.dram("projT", [ncol_tiles, 128, ntok], BF16, "ExternalOutput")
    if dbg:
        dbg_h = c.dram("dbg_h", [128, KD, TP], BF16, "ExternalOutput")
        dbg_r = c.dram("dbg_r", [128, TP], F32, "ExternalOutput")
        dbg_w = c.dram("dbg_w", [2, 128, 2048], BF16, "ExternalOutput")
    tc = TokCommon(ctx)
    xT = c.sb("xTs", [128, KD, TP], F32)
    xTB = [Buf(f"xT{m}") for m in range(KD)]
    xS = c.dsem("xload")
    hT = c.sb("hT", [128, KD, TP], BF16)
    hTB = [Buf(f"hT{m}") for m in range(KD)]
    gcol = c.sb("gcol_s", [128, 16], F32)
    gcolB = Buf("gcol")
    gS = c.dsem("gload")
    c.sp.dma(lambda e: e.dma_start(out=gcol[:], in_=g_d[:, :]), gS, writes=[gcolB])
    ob = [c.sb(f"ob{i}", [128, TP], BF16) for i in range(2)]
    obB = [Buf(f"ob{i}") for i in range(2)]
    obS = [c.dsem(f"ob{i}") for i in range(2)]
    oi = [0]
    for ps_ in range(ntok // TP):
        t0 = ps_ * TP
        fin = xS.count + KD
        for m in range(KD):
            c.sp.dma(lambda e, m=m, t0=t0: e.dma_start(out=xT[:, m, :], in_=xT_d[m, :, t0:t0 + TP]), xS, writes=[xTB[m]], final=fin)
        tc.norm(xT, xTB, gcol, gcolB, 0, hT, hTB)

        def rhs_fn(kc, tb):
            return hT[:, kc, tb * 512:(tb + 1) * 512], [hTB[kc]]

        def evac(j, ps, psB, t0=t0):
            b = oi[0] % 2
            oi[0] += 1
            sc = (128 ** -0.5) if j < 8 else 1.0
            c.act.op(lambda e, b=b, sc=sc: e.activation(out=ob[b][:], in_=ps[:], func=AF.Copy, scale=sc),
                     reads=[psB], writes=[obB[b]])
            c.act.dma(lambda e, b=b, j=j: e.dma_start(out=out_d[j, :, t0:t0 + TP], in_=ob[b][:]), obS[b], reads=[obB[b]])
        tc.linear(w_d, KD, ncol_tiles, rhs_fn, evac)
        if dbg:
            dS = c.dsem("dbg")
            c.sp.dma(lambda e: e.dma_start(out=dbg_h[:, :, :], in_=hT[:]), dS, reads=hTB)
            c.sp.dma(lambda e: e.dma_start(out=dbg_r[:, :], in_=tc.rstd[:]), dS, reads=[tc.rstdB])
            c.sp.dma(lambda e: e.dma_start(out=dbg_w[0], in_=tc.wbf[0][:]), dS, reads=[tc.wbfB[0]])
            c.sp.dma(lambda e: e.dma_start(out=dbg_w[1], in_=tc.wbf[1][:]), dS, reads=[tc.wbfB[1]])
    return ctx.finish()


def build_phaseC(ntok, moe, final):
    ctx = Ctx()
    c = ctx
    NS = 8 if moe else 4
    xT_d = c.dram("xT", [16, 128, ntok], F32, "ExternalInput")
    g_d = c.dram("gcol", [128, 48], F32, "ExternalInput")
    oT_d = c.dram("oT", [8, 128, ntok], BF16, "ExternalInput")
    yT_d = c.dram("yT", [8, 128, ntok], BF16, "ExternalInput")
    wglu_d = c.dram("w_glu", [8, 128, 8 * 128], F32, "ExternalInput")
    pat_d = c.dram("p_attn", [16, 128, 8 * 128], F32, "ExternalInput")
    pss_d = c.dram("p_ssm", [16, 128, 8 * 128], F32, "ExternalInput")
    wg_d = c.dram("w_g", [32, 128, KD * 128], F32, "ExternalInput")
    wo_d = c.dram("w_out", [16, 128, KD * 128], F32, "ExternalInput")
    fg_d = c.dram("f_gate", [NS * 8, 128, KD * 128], F32, "ExternalInput")
    fu_d = c.dram("f_up", [NS * 8, 128, KD * 128], F32, "ExternalInput")
    fd_d = c.dram("f_down", [NS * 16, 128, 8 * 128], F32, "ExternalInput")
    if moe:
        wr_d = c.dram("w_router", [128, KD * 8], F32, "ExternalInput")
        id_d = c.dram("ident", [128, 128], BF16, "ExternalInput")
    out_d = c.dram("outT", [16, 128, ntok], F32, "ExternalOutput")
    tc = TokCommon(ctx, 3, 2, True) if moe else TokCommon(ctx)
    mul, add = ALU.mult, ALU.add
    XBUF = c.sb("XBUF", [128, KD, TP], F32)
    XB16 = XBUF[:].bitcast(BF16).rearrange("p (a t) -> p a t", t=TP) if False else XBUF[:].bitcast(BF16)
    def x16(i, lo=0, hi=TP):
        return XB16[:, i // 2, (i % 2) * TP + lo:(i % 2) * TP + hi]
    xTB = [Buf(f"xT{m}") for m in range(KD)]
    xS = c.dsem("xload")
    oyS = c.dsem("oyload")
    stS = [c.dsem("xst0"), c.dsem("xst1")]
    HB = c.sb("HBUF", [128, KD, TP], BF16)
    HBB = [Buf(f"hT{m}") for m in range(KD)]
    MB = c.sb("MBUF", [128, KD, TP], BF16)
    MBB = [Buf(f"mb{m}") for m in range(KD)]
    gcol = c.sb("gcol_s", [128, 48], F32)
    gcolB = Buf("gcol")
    gS = c.dsem("gload")
    c.sp.dma(lambda e: e.dma_start(out=gcol[:], in_=g_d[:, :]), gS, writes=[gcolB])
    S1 = c.sb("S1", [128, TP], F32); S1B = Buf("S1")
    S2 = c.sb("S2", [128, TP], F32); S2B = Buf("S2")
    T1 = c.sb("T1", [128, TP], F32); T1B = Buf("T1")
    T2 = c.sb("T2", [128, TP], F32); T2B = Buf("T2")
    sgb = c.sb("sgb", [128, TP], BF16); sgbB = Buf("sgb")
    if moe:
        wrs = c.sb("wr_st", [128, KD * 8], F32); wrb = c.sb("wr_bf", [128, KD * 8], BF16); wrB = Buf("wr")
        ident = c.sb("ident_s", [128, 128], BF16)
        c.sp.dma(lambda e: e.dma_start(out=wrs[:], in_=wr_d[:, :]), gS, writes=[wrB], final=3)
        c.sp.dma(lambda e: e.dma_start(out=ident[:], in_=id_d[:, :]), gS, writes=[wrB], final=3)
        gcolB.lw = (gS, 3)
        c.dve.op(lambda e: e.tensor_copy(out=wrb[:], in_=wrs[:]), reads=[wrB], writes=[wrB])
        lg = c.sb("lg", [128, 8, 8], F32); lgB = Buf("lg")
        m8 = c.sb("m8", [128, 8, 8], F32)
        gg = c.sb("gg", [128, 8, 2], F32)
        wts = c.sb("wts", [128, 8, 8], F32)
        wl = c.sb("wl", [128, 8, 128], BF16); wlB = Buf("wl")
        wbc = c.sb("wbc", [128, 8, TP], BF16); wbcB = Buf("wbc")

    for ps_ in range(ntok // TP):
        t0 = ps_ * TP

        def load_x(t0=t0):
            fin = xS.count + KD
            for m in range(KD):
                c.sp.dma(lambda e, m=m, t0=t0: e.dma_start(out=XBUF[:, m, :], in_=xT_d[m, :, t0:t0 + TP]), xS, writes=[xTB[m]], final=fin)
        load_x()
        tc.norm(XBUF, xTB, gcol, gcolB, 0, HB, HBB)
        fin = oyS.count + 16
        for i in range(8):
            c.sp.dma(lambda e, i=i, t0=t0: e.dma_start(out=x16(i), in_=oT_d[i, :, t0:t0 + TP]), oyS, writes=[xTB[i // 2]], final=fin)
            c.sp.dma(lambda e, i=i, t0=t0: e.dma_start(out=x16(8 + i), in_=yT_d[i, :, t0:t0 + TP]), oyS, writes=[xTB[(8 + i) // 2]], final=fin)

        def ev_glu(j, ps, psB):
            c.act.op(lambda e: e.activation(out=sgb[:], in_=ps[:], func=AF.Sigmoid), reads=[psB], writes=[sgbB])
            c.dve.op(lambda e: e.tensor_tensor(out=x16(16 + j), in0=x16(8 + j), in1=sgb[:], op=mul),
                     reads=[sgbB, xTB[(8 + j) // 2]], writes=[xTB[(16 + j) // 2]])
        tc.linear(wglu_d, 8, 8, lambda kc, tb: (x16(8 + kc, tb * 512, (tb + 1) * 512), [xTB[(8 + kc) // 2]]), ev_glu)
        rhs_h = lambda kc, tb: (HB[:, kc, tb * 512:(tb + 1) * 512], [HBB[kc]])
        for m in range(KD):
            def ev_ga(j, ps, psB):
                c.act.op(lambda e: e.activation(out=S1[:], in_=ps[:], func=AF.Sigmoid), reads=[psB], writes=[S1B])
            tc.linear(wg_d, KD, 1, rhs_h, ev_ga, j0=m)
            def ev_a(j, ps, psB):
                c.dve.op(lambda e: e.tensor_tensor(out=T1[:], in0=ps[:], in1=S1[:], op=mul), reads=[psB, S1B], writes=[T1B])
            tc.linear(pat_d, 8, 1, lambda kc, tb: (x16(kc, tb * 512, (tb + 1) * 512), [xTB[kc // 2]]), ev_a, j0=m)
            def ev_gb(j, ps, psB):
                c.act.op(lambda e: e.activation(out=S2[:], in_=ps[:], func=AF.Sigmoid), reads=[psB], writes=[S2B])
            tc.linear(wg_d, KD, 1, rhs_h, ev_gb, j0=16 + m)
            def ev_b(j, ps, psB, m=m):
                c.dve.op(lambda e: e.tensor_tensor(out=T2[:], in0=ps[:], in1=S2[:], op=mul), reads=[psB, S2B], writes=[T2B])
                c.pool.op(lambda e: e.tensor_tensor(out=MB[:, m, :], in0=T1[:], in1=T2[:], op=add), reads=[T1B, T2B], writes=[MBB[m]])
            tc.linear(pss_d, 8, 1, lambda kc, tb: (x16(16 + kc, tb * 512, (tb + 1) * 512), [xTB[(16 + kc) // 2]]), ev_b, j0=m)
        load_x()
        def ev_res(j, ps, psB):
            mm = j % 16
            c.dve.op(lambda e: e.tensor_tensor(out=XBUF[:, mm, :], in0=ps[:], in1=XBUF[:, mm, :], op=add), reads=[psB, xTB[mm]], writes=[xTB[mm]])
        tc.linear(wo_d, KD, 16, lambda kc, tb: (MB[:, kc, tb * 512:(tb + 1) * 512], [MBB[kc]]), ev_res)
        tc.norm(XBUF, xTB, gcol, gcolB, 1, HB, HBB)
        if moe:
            ps, psB = tc.next_pt()
            for tt in range(TP // 128):
                for kc in range(KD):
                    c.pe.op(lambda e, tt=tt, kc=kc, ps=ps: e.matmul(ps[:, tt * 8:(tt + 1) * 8], HB[:, kc, tt * 128:(tt + 1) * 128], wrb[:, kc * 8:(kc + 1) * 8],
                                                                  start=(kc == 0), stop=(kc == KD - 1)),
                            reads=[HBB[kc], wrB], writes=[psB], last=(kc == KD - 1))
            c.dve.op(lambda e, ps=ps: e.tensor_copy(out=lg[:], in_=ps[:, 0:64].rearrange("p (a b) -> p a b", b=8)), reads=[psB], writes=[lgB])
            for tt in range(TP // 128):
                c.dve.op(lambda e, tt=tt: e.max(out=m8[:, tt, :], in_=lg[:, tt, :]), reads=[lgB], writes=[lgB])
            c.dve.op(lambda e: e.tensor_tensor(out=gg[:, :, 0], in0=m8[:, :, 0], in1=m8[:, :, 1], op=ALU.subtract), reads=[lgB], writes=[lgB])
            c.act.op(lambda e: e.activation(out=gg[:, :, 0], in_=gg[:, :, 0], func=AF.Sigmoid), reads=[lgB], writes=[lgB])
            c.dve.op(lambda e: e.tensor_scalar(out=gg[:, :, 1], in0=gg[:, :, 0], scalar1=-1.0, scalar2=1.0, op0=mul, op1=add), reads=[lgB], writes=[lgB])
            for tt in range(TP // 128):
                c.dve.op(lambda e, tt=tt: e.tensor_scalar(out=wts[:, tt, :], in0=lg[:, tt, :], scalar1=m8[:, tt, 0:1], scalar2=gg[:, tt, 0:1],
                                                          op0=ALU.is_equal, op1=mul), reads=[lgB], writes=[lgB])
                c.dve.op(lambda e, tt=tt: e.tensor_scalar(out=m8[:, tt, :], in0=lg[:, tt, :], scalar1=m8[:, tt, 1:2], scalar2=gg[:, tt, 1:2],
                                                          op0=ALU.is_equal, op1=mul), reads=[lgB], writes=[lgB])
                c.dve.op(lambda e, tt=tt: e.tensor_tensor(out=wts[:, tt, :], in0=wts[:, tt, :], in1=m8[:, tt, :], op=add), reads=[lgB], writes=[lgB])
            for tt in range(TP // 128):
                c.dve.op(lambda e, tt=tt: e.tensor_copy(out=wl[:], in_=wts[:, tt, :].unsqueeze(2).to_broadcast([128, 8, 128])), reads=[lgB], writes=[wlB])
                for e_ in range(8):
                    pw, pwB = tc.pt[(tc.pti + e_ // 2) % 4], tc.ptB[(tc.pti + e_ // 2) % 4]
                ps2, ps2B = tc.next_pt()
                for e_ in range(8):
                    c.pe.op(lambda e, e_=e_, ps2=ps2: e.matmul(ps2[:, e_ * 128:(e_ + 1) * 128], wl[:, e_, :], ident[:], start=True, stop=True),
                            reads=[wlB, wrB], writes=[ps2B], last=(e_ == 7))
                c.act.op(lambda e, tt=tt, ps2=ps2: e.activation(out=wbc[:, :, tt * 128:(tt + 1) * 128], in_=ps2[:, 0:1024].rearrange("p (a b) -> p a b", b=128), func=AF.Copy),
                         reads=[ps2B], writes=[wbcB])
        for s in range(NS):
            for f in range(8):
                def ev_g(j, ps, psB):
                    c.act.op(lambda e: e.activation(out=S1[:], in_=ps[:], func=AF.Silu), reads=[psB], writes=[S1B])
                tc.linear(fg_d, KD, 1, rhs_h, ev_g, j0=s * 8 + f)
                def ev_u(j, ps, psB, f=f, s=s):
                    if moe:
                        c.dve.op(lambda e: e.tensor_tensor(out=T1[:], in0=ps[:], in1=S1[:], op=mul), reads=[psB, S1B], writes=[T1B])
                        c.pool.op(lambda e: e.tensor_tensor(out=MB[:, f, :], in0=T1[:], in1=wbc[:, s, :], op=mul), reads=[T1B, wbcB], writes=[MBB[f]])
                    else:
                        c.dve.op(lambda e: e.tensor_tensor(out=MB[:, f, :], in0=ps[:], in1=S1[:], op=mul), reads=[psB, S1B], writes=[MBB[f]])
                tc.linear(fu_d, KD, 1, rhs_h, ev_u, j0=s * 8 + f)
            tc.linear(fd_d, 8, 16, lambda kc, tb: (MB[:, kc, tb * 512:(tb + 1) * 512], [MBB[kc]]), ev_res, j0=s * 16)
        if final:
            tc.norm_stats(XBUF, xTB)
        for m in range(KD):
            if final:
                b = m % 2
                tb_, tbB = (T1, T1B) if b == 0 else (T2, T2B)
                c.dve.op(lambda e, m=m, tb_=tb_: e.scalar_tensor_tensor(out=tb_[:], in0=XBUF[:, m, :], scalar=gcol[:, 32 + m:33 + m], in1=tc.rstd[:], op0=mul, op1=mul),
                         reads=[xTB[m], gcolB, tc.rstdB], writes=[tbB])
                c.sp.dma(lambda e, m=m, tb_=tb_, t0=t0: e.dma_start(out=out_d[m, :, t0:t0 + TP], in_=tb_[:]), stS[b], reads=[tbB])
            else:
                c.sp.dma(lambda e, m=m, t0=t0: e.dma_start(out=out_d[m, :, t0:t0 + TP], in_=XBUF[:, m, :]), stS[m % 2], reads=[xTB[m]], final=stS[m % 2].count + (KD - m + 1) // 2)
    return ctx.finish()

S = 16384
NQB = S // 512


def attn_consts():
    i = np.arange(128)
    ntri = -(i[:, None] >= i[None, :]).astype(np.float32)
    mask = (i[:, None] < i[None, :]).astype(np.float32)
    arrs = [np.eye(128, dtype=np.float32), ntri, -np.ones((128, 128), np.float32), mask, np.zeros((128, 128), np.float32)]
    return {"cpack": np.ascontiguousarray(np.stack(arrs, axis=1)).astype(NPBF)}


def emit_attention(ctx, qT_d, kT_d, vT_d, oT_d, pss, tps, nqb=NQB, stages=5, alias=None, tick=None):
    c = ctx
    nk = nqb * 4
    L = nqb * 512
    qT = c.sb("qT_s", [128, L], BF16)
    kT = c.sb("kT_s", [128, L], BF16)
    if alias is None:
        vtok = c.sb("vtok_s", [128, nk, 128], BF16)
        aB = []
    else:
        vtok = alias[0][:].rearrange("p (a b) -> p a b", b=128)
        aB = [alias[1]]
    qB, kB, vTB = Buf("qT"), Buf("kT"), Buf("vT")
    vtokB = [Buf(f"vtok{i}") for i in range(nk)]
    ldS = c.dsem("attn_ld")
    cB = Buf("attn_consts")
    cS = c.dsem("attn_c")
    cd = c.dram("cpack", [128, 5, 128], BF16, "ExternalInput")
    cp = c.sb("cpack_s", [128, 5, 128], BF16)
    c.sp.dma(lambda e: e.dma_start(out=cp[:], in_=cd[:, :, :]), cS, writes=[cB])
    ident, ntri, nones, mask, zeros = [cp[:, i, :] for i in range(5)]
    nch = max(1, L // 4096)
    cw = L // nch
    for (sb_, d_, B_, nm_) in [(qT, qT_d, qB, "attn_q"), (kT, kT_d, kB, "attn_k")]:
        gS_ = c.dsem(nm_)
        for i in range(nch):
            c.sp.dma(lambda e, sb_=sb_, d_=d_, i=i: e.dma_start(out=sb_[:, i * cw:(i + 1) * cw], in_=d_[:, i * cw:(i + 1) * cw]),
                     gS_, writes=[B_], final=nch)
    fin = nch
    tw = nk // nch
    for i in range(nch):
        c.sp.dma(lambda e, i=i: e.dma_start(out=vtok[:, i * tw:(i + 1) * tw, :], in_=vT_d[:, i * tw:(i + 1) * tw, :]),
                 ldS, writes=vtokB[i * tw:(i + 1) * tw] + aB, final=fin)
    NST = 2
    eb = [c.sb(f"eb{i}", [128, 512], F32) for i in range(NST)]
    ebB = [Buf(f"eb{i}") for i in range(NST)]
    spb = [c.sb(f"spb{i}", [128, 512], BF16) for i in range(NST)]
    spB = [Buf(f"spb{i}") for i in range(NST)]
    wb = [c.sb(f"wb{i}", [128, 512], BF16) for i in range(NST)]
    wbB = [Buf(f"wb{i}") for i in range(NST)]
    acc = [c.sb(f"acc{i}", [128, 512], BF16) for i in range(2)]
    accB = [Buf(f"acc{i}") for i in range(2)]
    osb = [c.sb(f"osb{i}", [128, 512], BF16) for i in range(2)]
    osbB = [Buf(f"osb{i}") for i in range(2)]
    osS = [c.dsem(f"osb{i}") for i in range(2)]
    zps = pss[0:2]
    wps = pss[2:4]
    ops = pss[4:5]

    items = []
    for i in range(nqb):
        for p in range(4 * i + 3, -1, -1):
            jj = p - 4 * i
            c0 = 128 * jj if jj >= 0 else 0
            items.append((i, p, c0, jj >= 0, p == 4 * i + 3, p == 0))
    N = len(items)

    def st1(n):
        i, p, c0, diag, first, lastp = items[n]
        ps, psB = zps[n % 2]
        t0 = i * 512
        c.pe.op(lambda e, ps=ps, p=p, c0=c0, t0=t0: e.matmul(ps[:, c0:512], kT[:, p * 128:(p + 1) * 128], qT[:, t0 + c0:t0 + 512],
                                                             start=True, stop=True),
                reads=[kB, qB], writes=[psB])

    def st2(n):
        i, p, c0, diag, first, lastp = items[n]
        ps, psB = zps[n % 2]
        s = n % NST
        c.act.op(lambda e, ps=ps, s=s, c0=c0: e.activation(out=eb[s][:, c0:512], in_=ps[:, c0:512], func=AF.Exp),
                 reads=[psB], writes=[ebB[s]])
        c.act.op(lambda e, s=s, c0=c0: e.activation(out=spb[s][:, c0:512], in_=eb[s][:, c0:512], func=AF.Ln, bias=1.0),
                 reads=[ebB[s]], writes=[spB[s]])
        if diag:
            c.pool.op(lambda e, s=s, c0=c0: e.tensor_tensor(out=spb[s][:, c0:c0 + 128], in0=spb[s][:, c0:c0 + 128], in1=mask, op=ALU.mult),
                      reads=[spB[s], cB], writes=[spB[s]])

    def st3(n):
        i, p, c0, diag, first, lastp = items[n]
        ps, psB = wps[n % 2]
        s = n % NST
        a = i % 2
        t0 = i * 512
        c.pe.op(lambda e, ps=ps, p=p, c0=c0, t0=t0: e.matmul(ps[:, c0:512], kT[:, p * 128:(p + 1) * 128], qT[:, t0 + c0:t0 + 512],
                                                             start=True, stop=False),
                reads=[kB, qB], writes=[psB], last=False)
        c.pe.op(lambda e, ps=ps, s=s, c0=c0: e.matmul(ps[:, c0:512], ntri, spb[s][:, c0:512], start=False, stop=first),
                reads=[cB, spB[s]], writes=[psB], last=first)
        if not first:
            c.pe.op(lambda e, ps=ps, a=a, c0=c0: e.matmul(ps[:, c0:512], nones, acc[a][:, c0:512], start=False, stop=True),
                    reads=[cB, accB[a]], writes=[psB])

    def st4(n):
        i, p, c0, diag, first, lastp = items[n]
        ps, psB = wps[n % 2]
        s = n % NST
        a = i % 2
        c.act.op(lambda e, ps=ps, s=s, c0=c0: e.activation(out=wb[s][:, c0:512], in_=ps[:, c0:512], func=AF.Exp),
                 reads=[psB], writes=[wbB[s]])
        if diag:
            c.pool.op(lambda e, s=s, c0=c0: e.tensor_tensor(out=wb[s][:, c0:c0 + 128], in0=wb[s][:, c0:c0 + 128], in1=mask, op=ALU.mult),
                      reads=[wbB[s], cB], writes=[wbB[s]])
        if not lastp:
            if first:
                c.pool.op(lambda e, a=a: e.memset(acc[a][:], 0.0), writes=[accB[a]])
            c.pool.op(lambda e, a=a, s=s, c0=c0: e.tensor_tensor(out=acc[a][:, c0:512], in0=acc[a][:, c0:512], in1=spb[s][:, c0:512], op=ALU.add),
                      reads=[accB[a], spB[s]], writes=[accB[a]])

    def st5(n):
        i, p, c0, diag, first, lastp = items[n]
        ps, psB = ops[0]
        s = n % NST
        if first:
            c.pe.op(lambda e, ps=ps: e.matmul(ps[:, 0:512], zeros, qT[:, 0:512], start=True, stop=False),
                    reads=[cB, qB], writes=[psB], last=False)
        c.pe.op(lambda e, ps=ps, p=p, s=s, c0=c0: e.matmul(ps[:, c0:512], vtok[:, p, :], wb[s][:, c0:512], start=False, stop=lastp),
                reads=[vtokB[p], wbB[s]], writes=[psB])
        if lastp:
            o = i % 2
            t0 = i * 512
            c.act.op(lambda e, ps=ps, o=o: e.activation(out=osb[o][:], in_=ps[:, 0:512], func=AF.Copy), reads=[psB], writes=[osbB[o]])
            c.act.dma(lambda e, o=o, t0=t0: e.dma_start(out=oT_d[:, t0:t0 + 512], in_=osb[o][:]), osS[o], reads=[osbB[o]])

    if stages == -1:
        return
    if stages == 0:
        c.sp.dma(lambda e: e.dma_start(out=oT_d[:, 0:512], in_=vtok[:, 0:4, :].rearrange('p a b -> p (a b)')), osS[0], reads=vtokB)
        return
    for n in range(N + 3):
        if tick is not None:
            tick(n / float(N))
        if n < N and stages >= 1:
            st1(n)
        if 0 <= n - 1 < N and stages >= 2:
            st2(n - 1)
        if 0 <= n - 2 < N and stages >= 3:
            st3(n - 2)
            if stages >= 4:
                st4(n - 2)
        if 0 <= n - 3 < N and stages >= 5:
            st5(n - 3)

S = 16384
BW = 512
NSTEP = 9
PKW = 12 + 128 + 1 + 192 + 128 + 512 + 512


def ssm_pack(a_re, a_im, log_dt, b_re, b_im, c_re, c_im, dsk, h):
    G = slice(8 * h, 8 * h + 8)
    are, aim, ldt = a_re[G], a_im[G], log_dt[G]
    bre, bim, cre, cim, dd = b_re[G], b_im[G], c_re[G], c_im[G], dsk[G]
    l1 = lambda t: t.reshape(4, 2, 64).transpose(1, 2, 0).reshape(128, 4)
    small = np.stack([l1(are), l1(aim), l1(np.repeat(ldt[:, None], 64, 1))], axis=2).reshape(128, 12)
    c1 = lambda t: np.tile(t.transpose(0, 2, 1).reshape(4, 2, 64, 16).transpose(1, 2, 0, 3).reshape(128, 4, 1, 16), (1, 1, 8, 1)).reshape(128, 4 * 128)
    dcol = dd.reshape(128, 1)
    rep = lambda t: np.repeat(t, 16, axis=0)
    p2 = np.concatenate([rep(are), rep(aim), rep(np.repeat(ldt[:, None], 64, 1))], axis=1)
    b2 = lambda t: t.transpose(0, 2, 1).reshape(128, 64)
    ch = np.arange(128)
    r = np.arange(128)
    m1 = np.zeros((128, 4, 128), np.float32)
    m2 = np.zeros((128, 4, 128), np.float32)
    for m in range(4):
        for g2 in range(2):
            rows = (ch // 32 == m) & ((ch // 16) % 2 == g2)
            m1[np.ix_(rows, [m], np.arange(64 * g2, 64 * g2 + 64))] = 1.0
            rr = (r // 64 == g2)
            cols = (ch // 16 == 2 * m + g2)
            m2[np.ix_(rr, [m], np.nonzero(cols)[0])] = 1.0
    pk = np.concatenate([small, c1(cre)[:, :0], dcol * 0 + 0, ], axis=1) if False else None
    pack = np.concatenate([small, np.concatenate([b2(bre), b2(bim)], axis=1), dcol, p2,
                           np.zeros((128, 128), np.float32), m1.reshape(128, 512), m2.reshape(128, 512)], axis=1).astype(np.float32)
    cpk = np.concatenate([c1(cre), c1(cim)], axis=1).astype(np.float32)
    assert pack.shape[1] == PKW, pack.shape
    return pack, cpk


class T:
    def __init__(self, ctx):
        self.c = ctx
        self.n = 0
        self.free = {}

    def new(self, shape):
        key = tuple(int(x) for x in shape)
        if self.free.get(key):
            return self.free[key].pop()
        self.n += 1
        return (self.c.sb(f"sst{self.n}", list(key), F32), Buf(f"sst{self.n}"))

    def rel(self, *tiles):
        for x in tiles:
            if hasattr(x[0], "name"):
                self.free.setdefault(tuple(int(v) for v in x[0].shape), []).append(x)

    def tt(self, a, b, op, shape=None, out=None):
        o = out or self.new(shape or list(a[0].shape))
        self.c.dve.op(lambda e: e.tensor_tensor(out=o[0][:], in0=a[0][:], in1=b[0][:], op=op), reads=[a[1], b[1]], writes=[o[1]])
        return o

    def ts(self, a, s1, op0, s2=None, op1=None, out=None):
        o = out or self.new(list(a[0].shape))
        if op1 is None:
            self.c.dve.op(lambda e: e.tensor_scalar(out=o[0][:], in0=a[0][:], scalar1=s1, scalar2=None, op0=op0), reads=[a[1]], writes=[o[1]])
        else:
            self.c.dve.op(lambda e: e.tensor_scalar(out=o[0][:], in0=a[0][:], scalar1=s1, scalar2=s2, op0=op0, op1=op1), reads=[a[1]], writes=[o[1]])
        return o

    def act(self, a, func, scale=1.0, out=None):
        o = out or self.new(list(a[0].shape))
        self.c.act.op(lambda e: e.activation(out=o[0][:], in_=a[0][:], func=func, scale=scale), reads=[a[1]], writes=[o[1]])
        return o

    def recip(self, a):
        o = self.new(list(a[0].shape))
        self.c.dve.op(lambda e: e.reciprocal(out=o[0][:], in_=a[0][:]), reads=[a[1]], writes=[o[1]])
        return o


def ssm_params(t, are, aim, ldt, want_z):
    mul, add, sub = ALU.mult, ALU.add, ALU.subtract
    dtv = t.act(ldt, AF.Exp)
    tre = t.tt(dtv, are, mul)
    ang = t.tt(dtv, aim, mul)
    t.rel(dtv)
    mag = t.act(tre, AF.Exp)
    t.rel(tre)
    s = t.act(ang, AF.Sin, scale=1.0 / 64)
    sh = t.act(ang, AF.Sin, scale=1.0 / 128)
    t.rel(ang)
    sh2 = t.tt(sh, sh, mul)
    c = t.ts(sh2, -2.0, mul, 1.0, add)
    t.rel(sh, sh2)
    for _ in range(6):
        sc = t.tt(s, c, mul)
        cc = t.tt(c, c, mul)
        ss = t.tt(s, s, mul)
        t.rel(s, c)
        s = t.ts(sc, 2.0, mul)
        c = t.tt(cc, ss, sub)
        t.rel(sc, cc, ss)
    abr = t.tt(mag, c, mul)
    abi = t.tt(mag, s, mul)
    t.rel(mag, c, s)
    if not want_z:
        return abr, abi, None, None
    a2 = t.tt(are, are, mul)
    b2 = t.tt(aim, aim, mul)
    den = t.tt(a2, b2, add)
    rden = t.recip(den)
    t.rel(a2, b2, den)
    nre = t.ts(abr, -1.0, add)
    p1 = t.tt(nre, are, mul)
    p2 = t.tt(abi, aim, mul)
    p3 = t.tt(p1, p2, add)
    zr = t.tt(p3, rden, mul)
    t.rel(p1, p2, p3)
    p1 = t.tt(abi, are, mul)
    p2 = t.tt(nre, aim, mul)
    p3 = t.tt(p1, p2, sub)
    zi = t.tt(p3, rden, mul)
    t.rel(p1, p2, p3, rden, nre)
    return abr, abi, zr, zi


def emit_ssm(ctx, uT_d, pack_d, cpk_d, yT_d, pss, nblk=S // BW):
    c = ctx
    t = T(ctx)
    mul, add, sub = ALU.mult, ALU.add, ALU.subtract
    L = nblk * BW
    pk = c.sb("ssm_pk", [128, PKW], F32)
    pkB = Buf("ssm_pk")
    pS = c.dsem("ssm_pk")
    c.sp.dma(lambda e: e.dma_start(out=pk[:], in_=pack_d[:, :]), pS, writes=[pkB])
    cpk = c.sb("ssm_cpk", [128, 1024], F32)
    cpkB = Buf("ssm_cpk")
    c.sp.dma(lambda e: e.dma_start(out=cpk[:], in_=cpk_d[:, :]), pS, writes=[cpkB], final=2)
    pkB.lw = (pS, 2)
    uT = c.sb("uT_s", [128, L], BF16)
    uB = Buf("uT")
    uS = c.dsem("ssm_u")
    nch = max(1, L // 4096)
    cw = L // nch
    for i in range(nch):
        c.sp.dma(lambda e, i=i: e.dma_start(out=uT[:, i * cw:(i + 1) * cw], in_=uT_d[:, i * cw:(i + 1) * cw]), uS, writes=[uB], final=nch)

    class V:
        def __init__(self, ap):
            self.ap = ap

        def __getitem__(self, k):
            return self.ap

        @property
        def shape(self):
            return self.ap.shape
    small = pk[:, 0:12].rearrange("p (m k) -> p m k", k=3)
    are1, aim1, ldt1 = [(V(small[:, :, k]), pkB) for k in range(3)]
    b2r = (V(pk[:, 12:76]), pkB)
    b2i = (V(pk[:, 76:140]), pkB)
    dcol = pk[:, 140:141]
    are2, aim2, ldt2 = [(V(pk[:, 141 + 64 * k:141 + 64 * (k + 1)]), pkB) for k in range(3)]
    m1 = pk[:, 461:973].rearrange("p (m k) -> p m k", k=128)
    m2 = pk[:, 973:1485].rearrange("p (m k) -> p m k", k=128)

    abr, abi, _, _ = ssm_params(t, are1, aim1, ldt1, False)
    pw_r = [abr]
    pw_i = [abi]
    for k in range(1, NSTEP):
        r_, i_ = pw_r[-1], pw_i[-1]
        rr = t.tt(r_, r_, mul)
        ii = t.tt(i_, i_, mul)
        ri = t.tt(r_, i_, mul)
        pw_r.append(t.tt(rr, ii, sub))
        pw_i.append(t.ts(ri, 2.0, mul))
        t.rel(rr, ii, ri)
    npw_i = [t.ts(x, -1.0, mul) for x in pw_i]
    _, _, zr2, zi2 = ssm_params(t, are2, aim2, ldt2, True)
    q1 = t.tt(zr2, b2r, mul)
    q2 = t.tt(zi2, b2i, mul)
    bbr = t.tt(q1, q2, sub)
    t.rel(q1, q2)
    q1 = t.tt(zr2, b2i, mul)
    q2 = t.tt(zi2, b2r, mul)
    bbi = t.tt(q1, q2, add)
    t.rel(q1, q2)
    BT = c.sb("ssm_BT", [128, 4, 2, 128], BF16)
    BTB = Buf("ssm_BT")
    for m in range(4):
        for ri, src in enumerate([bbr, bbi]):
            c.dve.op(lambda e, m=m, ri=ri, src=src: e.tensor_tensor(
                out=BT[:, m, ri, :].rearrange("p (a n) -> p a n", a=2), in0=m1[:, m, :].rearrange("p (a n) -> p a n", a=2),
                in1=src[0][:].unsqueeze(1).to_broadcast([128, 2, 64]), op=mul),
                reads=[pkB, src[1]], writes=[BTB])
    ZC = c.sb("ssm_ZC", [128, 4, 2, 128], BF16)
    ZCB = Buf("ssm_ZC")
    for m in range(4):
        c.dve.op(lambda e, m=m: e.tensor_tensor(out=ZC[:, m, 0, :], in0=m2[:, m, :], in1=cpk[:, m * 128:(m + 1) * 128], op=mul),
                 reads=[pkB, cpkB], writes=[ZCB])
        c.dve.op(lambda e, m=m: e.scalar_tensor_tensor(out=ZC[:, m, 1, :], in0=m2[:, m, :], scalar=-1.0, in1=cpk[:, 512 + m * 128:512 + (m + 1) * 128],
                                                       op0=mul, op1=mul),
                 reads=[pkB, cpkB], writes=[ZCB])

    XS = [[c.sb(f"ssm_X{p}{q}", [128, 2, BW], F32) for q in range(2)] for p in range(2)]
    XSB = [[[Buf(f"X{p}{q}{k}") for k in range(2)] for q in range(2)] for p in range(2)]
    XH = c.sb("ssm_XH", [128, 4, 2, BW], BF16)
    XHB = [Buf(f"XH{m}") for m in range(4)]
    CAR = c.sb("ssm_car", [128, 4, 4], F32)
    CARB = [Buf(f"car{m}") for m in range(4)]

    def hs_scan(ms, slots):
        cur = [0] * len(ms)
        for k in range(NSTEP):
            s_ = 1 << k
            first, second = [], []
            for idx, (m, sl) in enumerate(zip(ms, slots)):
                cu, ot = XS[sl][cur[idx]], XS[sl][1 - cur[idx]]
                cuB, otB = XSB[sl][cur[idx]], XSB[sl][1 - cur[idx]]
                pr = pw_r[k][0][:][:, m:m + 1]
                pi = pw_i[k][0][:][:, m:m + 1]
                npi = npw_i[k][0][:][:, m:m + 1]
                pB = [pw_r[k][1], pw_i[k][1], npw_i[k][1]]
                first.append((lambda e, cu=cu, ot=ot, s_=s_: e.tensor_copy(out=ot[:, 0, 0:s_], in_=cu[:, 0, 0:s_]), [cuB[0]], [otB[0]]))
                first.append((lambda e, cu=cu, ot=ot, s_=s_, pr=pr: e.scalar_tensor_tensor(
                    out=ot[:, 0, s_:BW], in0=cu[:, 0, 0:BW - s_], scalar=pr, in1=cu[:, 0, s_:BW], op0=mul, op1=add), [cuB[0]] + pB, [otB[0]]))
                first.append((lambda e, cu=cu, ot=ot, s_=s_: e.tensor_copy(out=ot[:, 1, 0:s_], in_=cu[:, 1, 0:s_]), [cuB[1]], [otB[1]]))
                first.append((lambda e, cu=cu, ot=ot, s_=s_, pr=pr: e.scalar_tensor_tensor(
                    out=ot[:, 1, s_:BW], in0=cu[:, 1, 0:BW - s_], scalar=pr, in1=cu[:, 1, s_:BW], op0=mul, op1=add), [cuB[1]] + pB, [otB[1]]))
                second.append((lambda e, cu=cu, ot=ot, s_=s_, npi=npi: e.scalar_tensor_tensor(
                    out=ot[:, 0, s_:BW], in0=cu[:, 1, 0:BW - s_], scalar=npi, in1=ot[:, 0, s_:BW], op0=mul, op1=add), [cuB[1], otB[0]] + pB, [otB[0]]))
                second.append((lambda e, cu=cu, ot=ot, s_=s_, pi=pi: e.scalar_tensor_tensor(
                    out=ot[:, 1, s_:BW], in0=cu[:, 0, 0:BW - s_], scalar=pi, in1=ot[:, 1, s_:BW], op0=mul, op1=add), [cuB[0], otB[1]] + pB, [otB[1]]))
                cur[idx] = 1 - cur[idx]
            for fn, rd, wr in first + second:
                c.dve.op(fn, reads=rd, writes=wr)
        return [(XS[sl][cur[i]], XSB[sl][cur[i]]) for i, sl in enumerate(slots)]

    ypre = c.sb("ssm_ypre", [128, BW], F32)
    ypB = Buf("ypre")
    g1 = c.sb("ssm_g1", [128, BW], F32)
    g1B = Buf("g1")
    yo = [c.sb(f"ssm_yo{i}", [128, BW], BF16) for i in range(2)]
    yoB = [Buf(f"yo{i}") for i in range(2)]
    yoS = [c.dsem(f"ssm_yo{i}") for i in range(2)]

    def stage_in(blk):
            t0 = blk * BW
            for pair in range(2):
                ms = [2 * pair, 2 * pair + 1]
                for sl, m in enumerate(ms):
                    psr, psrB = pss[0]
                    psi, psiB = pss[1]
                    c.pe.op(lambda e, m=m, psr=psr, t0=t0: e.matmul(psr[:, 0:BW], BT[:, m, 0, :], uT[:, t0:t0 + BW], start=True, stop=True),
                            reads=[BTB, uB], writes=[psrB])
                    c.pe.op(lambda e, m=m, psi=psi, t0=t0: e.matmul(psi[:, 0:BW], BT[:, m, 1, :], uT[:, t0:t0 + BW], start=True, stop=True),
                            reads=[BTB, uB], writes=[psiB])
                    c.dve.op(lambda e, psr=psr, sl=sl: e.tensor_copy(out=XS[sl][0][:, 0, :], in_=psr[:, 0:BW]), reads=[psrB], writes=[XSB[sl][0][0]])
                    c.dve.op(lambda e, psi=psi, sl=sl: e.tensor_copy(out=XS[sl][0][:, 1, :], in_=psi[:, 0:BW]), reads=[psiB], writes=[XSB[sl][0][1]])
                if blk > 0:
                    for sl, m in enumerate(ms):
                        ar_ = abr[0][:][:, m:m + 1]
                        ai_ = abi[0][:][:, m:m + 1]
                        for (dst, src_, sc_) in [(0, 0, ar_), (0, 2, ai_), (1, 1, ar_), (1, 0, ai_)]:
                            c.dve.op(lambda e, sl=sl, dst=dst, src_=src_, sc_=sc_, m=m: e.scalar_tensor_tensor(
                                out=XS[sl][0][:, dst, 0:1], in0=CAR[:, m, src_:src_ + 1], scalar=sc_, in1=XS[sl][0][:, dst, 0:1], op0=mul, op1=add),
                                reads=[XSB[sl][0][dst], CARB[m], abr[1], abi[1]], writes=[XSB[sl][0][dst]])
                res = hs_scan(ms, [0, 1])
                for (r_, rB), m in zip(res, ms):
                    c.dve.op(lambda e, r_=r_, m=m: e.tensor_copy(out=CAR[:, m, 0:2], in_=r_[:, :, BW - 1]), reads=rB, writes=[CARB[m]])
                    c.dve.op(lambda e, r_=r_, m=m: e.tensor_scalar(out=CAR[:, m, 2:3], in0=r_[:, 1, BW - 1:BW], scalar1=-1.0, scalar2=None, op0=mul),
                             reads=rB, writes=[CARB[m]])
                    c.dve.op(lambda e, r_=r_, m=m: e.tensor_copy(out=XH[:, m, :, :], in_=r_[:]), reads=rB, writes=[XHB[m]])
    def stage_out(blk):
            t0 = blk * BW
            py, pyB = pss[2]
            for m in range(4):
                for ri in range(2):
                    c.pe.op(lambda e, py=py, m=m, ri=ri: e.matmul(py[:, 0:BW], ZC[:, m, ri, :], XH[:, m, ri, :], start=(m == 0 and ri == 0), stop=(m == 3 and ri == 1)),
                            reads=[ZCB, XHB[m]], writes=[pyB], last=(m == 3 and ri == 1))
            c.dve.op(lambda e, py=py, t0=t0: e.scalar_tensor_tensor(out=ypre[:], in0=uT[:, t0:t0 + BW], scalar=dcol, in1=py[:, 0:BW], op0=mul, op1=add),
                     reads=[uB, pkB, pyB], writes=[ypB])
            c.pool.op(lambda e: e.tensor_tensor(out=g1[:], in0=ypre[:], in1=ypre[:], op=mul), reads=[ypB], writes=[g1B])
            c.pool.op(lambda e: e.tensor_scalar(out=g1[:], in0=g1[:], scalar1=0.044715, scalar2=1.0, op0=mul, op1=add), reads=[g1B], writes=[g1B])
            c.pool.op(lambda e: e.tensor_tensor(out=g1[:], in0=g1[:], in1=ypre[:], op=mul), reads=[g1B, ypB], writes=[g1B])
            c.act.op(lambda e: e.activation(out=g1[:], in_=g1[:], func=AF.Sigmoid, scale=1.5957691216057308), reads=[g1B], writes=[g1B])
            o_ = blk % 2
            c.pool.op(lambda e, o_=o_: e.tensor_tensor(out=yo[o_][:], in0=g1[:], in1=ypre[:], op=mul), reads=[g1B, ypB], writes=[yoB[o_]])
            c.pool.dma(lambda e, o_=o_, t0=t0: e.dma_start(out=yT_d[:, t0:t0 + BW], in_=yo[o_][:]), yoS[o_], reads=[yoB[o_]])

    def blocks():
        for blk in range(nblk):
            if blk > 0:
                stage_out(blk - 1)
            stage_in(blk)
            yield blk
        stage_out(nblk - 1)
    return blocks()


def build_phaseB(L=S):
    ctx = Ctx()
    c = ctx
    qd = c.dram("qT", [128, L], BF16, "ExternalInput")
    kd = c.dram("kT", [128, L], BF16, "ExternalInput")
    vd = c.dram("vtok", [128, L // 128, 128], BF16, "ExternalInput")
    ud = c.dram("uT", [128, L], BF16, "ExternalInput")
    pd = c.dram("spack", [128, PKW], F32, "ExternalInput")
    cd = c.dram("scpk", [128, 1024], F32, "ExternalInput")
    od = c.dram("oT", [128, L], BF16, "ExternalOutput")
    yd = c.dram("yT", [128, L], BF16, "ExternalOutput")
    pss = [(c.ps(f"pb{i}", [128, 512])[:], Buf(f"pb{i}")) for i in range(8)]
    nb = L // BW
    gen = emit_ssm(ctx, ud, pd, cd, yd, pss[5:8], nblk=nb)
    done = [0]

    def tick(frac):
        while done[0] < nb and done[0] < frac * nb + 1:
            next(gen)
            done[0] += 1
    emit_attention(ctx, qd, kd, vd, od, pss[:5], None, nqb=L // 512, tick=tick)
    for _ in gen:
        pass
    return ctx.finish()


_PROGS = {}


def _prog(key, fn):
    if key not in _PROGS:
        _PROGS[key] = fn()
    return _PROGS[key]


def _run(nc, in_maps):
    res = run_bass_kernel_spmd(nc, in_maps, core_ids=list(range(8)))
    return res.results


def kernel(**inp):
    NCORE = 8
    TOK = S // NCORE
    f32 = np.float32
    x = np.asarray(inp['x'], f32)[0]
    xT = [to_xT(x[c * TOK:(c + 1) * TOK]) for c in range(NCORE)]
    consts = attn_consts()
    ident = np.eye(128, dtype=f32).astype(NPBF)
    for layer in range(2):
        w_in = np.asarray(inp['w_in'][layer], f32)
        gA = gcols([np.asarray(inp['mix_norm'][layer], f32)])
        wq = wtiles(w_in[:, :4096])
        ncA = _prog('A', lambda: build_phaseA(TOK, 32))
        rA = _run(ncA, [{"xT": xT[c], "gcol": gA, "w_qkvu": wq} for c in range(NCORE)])
        P = [np.concatenate([rA[c]['projT'][j] for c in range(NCORE)], axis=1) for j in range(32)]
        del rA
        ncB = _prog('B', build_phaseB)
        mapsB = []
        for h in range(NCORE):
            pack, cpk = ssm_pack(*[np.asarray(inp[k][layer], f32) for k in
                                   ['ssm_a_re', 'ssm_a_im', 'ssm_log_dt', 'ssm_b_re', 'ssm_b_im', 'ssm_c_re', 'ssm_c_im', 'ssm_d']], h)
            vt = np.ascontiguousarray(P[16 + h].T.reshape(S // 128, 128, 128).transpose(1, 0, 2))
            mapsB.append({"qT": P[h], "kT": P[8 + h], "vtok": vt, "uT": P[24 + h], "spack": pack, "scpk": cpk, "cpack": consts["cpack"]})
        rB = _run(ncB, mapsB)
        del P, mapsB
        moe = (layer % 2 == 1)
        final = (layer == 1)
        gC = gcols([np.asarray(inp['mix_norm'][layer], f32), np.asarray(inp['ffn_norm'][layer], f32), np.asarray(inp['final_norm'], f32)])
        wC = {
            "gcol": gC,
            "w_glu": wtiles(np.asarray(inp['w_glu'][layer], f32)),
            "p_attn": wtiles(np.asarray(inp['p_attn'][layer], f32)),
            "p_ssm": wtiles(np.asarray(inp['p_ssm'][layer], f32)),
            "w_g": wtiles(w_in[:, 4096:]),
            "w_out": wtiles(np.asarray(inp['w_out'][layer], f32)),
        }
        i = layer // 2
        if not moe:
            wd = np.asarray(inp['ffn_w_down'][i], f32)
            wC["f_gate"] = wtiles(np.asarray(inp['ffn_w_gate'][i], f32))
            wC["f_up"] = wtiles(np.asarray(inp['ffn_w_up'][i], f32))
            wC["f_down"] = np.concatenate([wtiles(wd[1024 * s:1024 * (s + 1)]) for s in range(4)], axis=0)
        else:
            mg = np.asarray(inp['moe_w_gate'][i], f32)
            mu = np.asarray(inp['moe_w_up'][i], f32)
            md = np.asarray(inp['moe_w_down'][i], f32)
            wC["f_gate"] = np.concatenate([wtiles(mg[e]) for e in range(8)], axis=0)
            wC["f_up"] = np.concatenate([wtiles(mu[e]) for e in range(8)], axis=0)
            wC["f_down"] = np.concatenate([wtiles(md[e]) for e in range(8)], axis=0)
            wr = np.asarray(inp['w_router'][i], f32)
            wC["w_router"] = np.ascontiguousarray(wr.reshape(16, 128, 8).transpose(1, 0, 2).reshape(128, 128))
            wC["ident"] = ident
        ncC = _prog(('C', moe, final), lambda: build_phaseC(TOK, moe, final))
        mapsC = []
        for c in range(NCORE):
            m = dict(wC)
            m["xT"] = xT[c]
            m["oT"] = np.ascontiguousarray(np.stack([rB[hh]['oT'][:, c * TOK:(c + 1) * TOK] for hh in range(8)], axis=0))
            m["yT"] = np.ascontiguousarray(np.stack([rB[hh]['yT'][:, c * TOK:(c + 1) * TOK] for hh in range(8)], axis=0))
            mapsC.append(m)
        rC = _run(ncC, mapsC)
        xT = [np.asarray(rC[c]['outT'], f32) for c in range(NCORE)]
        del rB, rC, mapsC, wC
    out = np.concatenate([xT[c].reshape(2048, TOK).T for c in range(NCORE)], axis=0)
    return np.ascontiguousarray(out[None].astype(f32))
```

```python
import numpy as np
import ml_dtypes
import concourse.bass as bass
import concourse.mybir as mybir
from concourse.bass_utils import run_bass_kernel_spmd

F32 = mybir.dt.float32
BF16 = mybir.dt.bfloat16
AF = mybir.ActivationFunctionType
ALU = mybir.AluOpType
NPBF = ml_dtypes.bfloat16


class Sem:
    def __init__(self, nc, name, unit):
        self.h = nc.semaphore(name).__enter__()
        self.unit = unit
        self.count = 0


class Buf:
    __slots__ = ("name", "lw", "rd")

    def __init__(self, name):
        self.name = name
        self.lw = None
        self.rd = {}


class Stream:
    def __init__(self, ctx, name, sync_self):
        self.ctx = ctx
        self.name = name
        self.csem = Sem(ctx.nc, "c_" + name, 1)
        self.sync_self = sync_self
        self.known = {}
        self.items = []

    def _deps(self, reads, writes, ignore=None):
        deps = {}
        for b in reads:
            if b.lw is not None:
                s, v = b.lw
                if deps.get(s, 0) < v:
                    deps[s] = v
        for b in writes:
            if b.lw is not None:
                s, v = b.lw
                if deps.get(s, 0) < v:
                    deps[s] = v
            for s, v in b.rd.items():
                if deps.get(s, 0) < v:
                    deps[s] = v
        waits = []
        for s, v in deps.items():
            if s is self.csem and not self.sync_self:
                continue
            if ignore is not None and s is ignore[0] and v > ignore[1]:
                continue
            if self.known.get(s, 0) < v:
                waits.append((s, v))
                self.known[s] = v
        return waits

    def _mark(self, ev, reads, writes):
        s, v = ev
        for b in reads:
            if b.rd.get(s, 0) < v:
                b.rd[s] = v
        for b in writes:
            b.lw = ev
            b.rd = {}

    def op(self, fn, reads=(), writes=(), last=True):
        waits = self._deps(reads, writes)
        ev = (self.csem, self.csem.count + 1)
        if last:
            self.csem.count += 1
        self.items.append((waits, fn, ev if last else None))
        self._mark(ev, reads, writes)

    def dma(self, fn, sem, reads=(), writes=(), final=None):
        waits = self._deps(reads, writes, ignore=(sem, sem.count) if final is not None else None)
        sem.count += 1
        ev = (sem, sem.count)
        self.items.append((waits, fn, ev))
        self._mark((sem, final if final is not None else sem.count), reads, writes)

    def wait_for(self, sem):
        if sem.count > 0 and self.known.get(sem, 0) < sem.count:
            self.items.append(([(sem, sem.count)], None, None))
            self.known[sem] = sem.count

    def emit(self, eng):
        for waits, fn, ev in self.items:
            for s, v in waits:
                eng.wait_ge(s.h, v * s.unit)
            if fn is None:
                continue
            ins = fn(eng)
            if ev is not None:
                ins.then_inc(ev[0].h, ev[0].unit)
        self.items = []


class Ctx:
    def __init__(self):
        self.nc = bass.Bass("TRN2", target_bir_lowering=False)
        nc = self.nc
        self.pe = Stream(self, "pe", False)
        self.act = Stream(self, "act", True)
        self.dve = Stream(self, "dve", True)
        self.pool = Stream(self, "pool", True)
        self.sp = Stream(self, "sp", True)
        self.dsems = []
        self._n = 0

    def dsem(self, name=None):
        self._n += 1
        s = Sem(self.nc, name or f"d{self._n}", 16)
        self.dsems.append(s)
        return s

    def sb(self, name, shape, dt):
        return self.nc.sbuf_tensor(name, list(shape), dt).__enter__()

    def ps(self, name, shape, dt=F32):
        return self.nc.psum_tensor(name, list(shape), dt).__enter__()

    def dram(self, name, shape, dt, kind):
        return self.nc.dram_tensor(name, list(shape), dt, kind=kind).ap()

    def finish(self):
        for s in self.dsems:
            self.sp.wait_for(s)
        nc = self.nc
        with nc.Block() as block:
            block.sync(lambda e: self.sp.emit(e))
            block.tensor(lambda e: self.pe.emit(e))
            block.scalar(lambda e: self.act.emit(e))
            block.vector(lambda e: self.dve.emit(e))
            block.gpsimd(lambda e: self.pool.emit(e))
        return nc

D = 2048
KD = 16
TP = 1024
NTB = TP // 512
EPS = 1e-6


class TokCommon:
    def __init__(self, ctx, NW=4, NB=3, split_cast=False):
        self.ctx = ctx
        self.split_cast = split_cast
        c = ctx
        self.pt = [c.ps(f"pt{i}", [128, 1024]) for i in range(4)]
        self.ptB = [Buf(f"pt{i}") for i in range(4)]
        self.pti = 0
        self.NW, self.NB = NW, NB
        self.wst = [c.sb(f"wst{i}", [128, 2048], F32) for i in range(self.NW)]
        self.wstB = [Buf(f"wst{i}") for i in range(self.NW)]
        self.wstS = [c.dsem(f"wst{i}") for i in range(self.NW)]
        self.wbf = [c.sb(f"wbf{i}", [128, 2048], BF16) for i in range(self.NB)]
        self.wbfB = [Buf(f"wbf{i}") for i in range(self.NB)]
        self.wi = 0
        self.ones = c.sb("ones", [128, 128], BF16)
        self.onesB = Buf("ones")
        c.pool.op(lambda e: e.memset(self.ones[:], 1.0), writes=[self.onesB])
        self.sq = [c.sb(f"sq{i}", [128, TP], BF16) for i in range(2)]
        self.sqB = [Buf(f"sq{i}") for i in range(2)]
        self.rstd = c.sb("rstd", [128, TP], F32)
        self.rstdB = Buf("rstd")

    def next_pt(self):
        i = self.pti % 4
        self.pti += 1
        return self.pt[i], self.ptB[i]

    def norm(self, xT, xTB, gcol, gcolB, gidx, hT, hTB):
        c = self.ctx
        ps, psB = self.next_pt()
        for m in range(KD):
            s = m % 2
            c.act.op(lambda e, m=m, s=s: e.activation(out=self.sq[s][:], in_=xT[:, m, :], func=AF.Square),
                     reads=[xTB[m]], writes=[self.sqB[s]])
            for tb in range(NTB):
                c.pe.op(lambda e, m=m, s=s, tb=tb: e.matmul(ps[:, tb * 512:(tb + 1) * 512], self.ones[:],
                                                            self.sq[s][:, tb * 512:(tb + 1) * 512],
                                                            start=(m == 0), stop=(m == KD - 1)),
                        reads=[self.onesB, self.sqB[s]], writes=[psB], last=(tb == NTB - 1))
        c.dve.op(lambda e: e.tensor_scalar(out=self.rstd[:], in0=ps[:], scalar1=1.0 / D, scalar2=EPS,
                                           op0=ALU.mult, op1=ALU.add),
                 reads=[psB], writes=[self.rstdB])
        c.act.op(lambda e: e.activation(out=self.rstd[:], in_=self.rstd[:], func=AF.Ln),
                 reads=[self.rstdB], writes=[self.rstdB])
        c.act.op(lambda e: e.activation(out=self.rstd[:], in_=self.rstd[:], func=AF.Exp, scale=-0.5),
                 reads=[self.rstdB], writes=[self.rstdB])
        if hT is None:
            return
        for m in range(KD):
            c.dve.op(lambda e, m=m: e.scalar_tensor_tensor(out=hT[:, m, :], in0=xT[:, m, :],
                                                           scalar=gcol[:, gidx * KD + m:gidx * KD + m + 1],
                                                           in1=self.rstd[:], op0=ALU.mult, op1=ALU.mult),
                     reads=[xTB[m], gcolB, self.rstdB], writes=[hTB[m]])

    def norm_stats(self, xT, xTB):
        self.norm(xT, xTB, None, None, 0, None, None)

    def linear(self, Wt, KC, J, rhs_fn, evac_fn, j0=0):
        c = self.ctx
        for j in range(j0, j0 + J):
            a = self.wi % self.NW
            b = self.wi % self.NB
            self.wi += 1
            c.sp.dma(lambda e, j=j, a=a: e.dma_start(out=self.wst[a][:, :KC * 128], in_=Wt[j]),
                     self.wstS[a], writes=[self.wstB[a]])
            ceng = c.dve if (self.split_cast and self.wi % 2 == 1) else c.pool
            ceng.op(lambda e, a=a, b=b: e.tensor_copy(out=self.wbf[b][:, :KC * 128], in_=self.wst[a][:, :KC * 128]),
                    reads=[self.wstB[a]], writes=[self.wbfB[b]])
            ps, psB = self.next_pt()
            for kc in range(KC):
                for tb in range(NTB):
                    rap, rbufs = rhs_fn(kc, tb)
                    c.pe.op(lambda e, b=b, kc=kc, tb=tb, rap=rap, ps=ps: e.matmul(
                        ps[:, tb * 512:(tb + 1) * 512], self.wbf[b][:, kc * 128:(kc + 1) * 128], rap,
                        start=(kc == 0), stop=(kc == KC - 1)),
                        reads=[self.wbfB[b]] + rbufs, writes=[psB], last=(kc == KC - 1 and tb == NTB - 1))
            evac_fn(j, ps, psB)


def wtiles(W):
    K, N = W.shape
    return np.ascontiguousarray(W.reshape(K // 128, 128, N // 128, 128).transpose(2, 1, 0, 3).reshape(N // 128, 128, (K // 128) * 128))


def gcols(gs):
    return np.ascontiguousarray(np.concatenate([g.reshape(16, 128).T for g in gs], axis=1)).astype(np.float32)


def to_xT(xc):
    T = xc.shape[0]
    return np.ascontiguousarray(xc.T.reshape(16, 128, T))


def build_phaseA(ntok, ncol_tiles=32, dbg=False):
    ctx = Ctx()
    c = ctx
    xT_d = c.dram("xT", [16, 128, ntok], F32, "ExternalInput")
    g_d = c.dram("gcol", [128, 16], F32, "ExternalInput")
    w_d = c.dram("w_qkvu", [ncol_tiles, 128, KD * 128], F32, "ExternalInput")
    out_d = c.dram("projT", [ncol_tiles, 128, ntok], BF16, "ExternalOutput")
    if dbg:
        dbg_h = c.dram("dbg_h", [128, KD, TP], BF16, "ExternalOutput")
        dbg_r = c.dram("dbg_r", [128, TP], F32, "ExternalOutput")
        dbg_w = c.dram("dbg_w", [2, 128, 2048], BF16, "ExternalOutput")
    tc = TokCommon(ctx)
    xT = c.sb("xTs", [128, KD, TP], F32)
    xTB = [Buf(f"xT{m}") for m in range(KD)]
    xS = c.dsem("xload")
    hT = c.sb("hT", [128, KD, TP], BF16)
    hTB = [Buf(f"hT{m}") for m in range(KD)]
    gcol = c.sb("gcol_s", [128, 16], F32)
    gcolB = Buf("gcol")
    gS = c.dsem("gload")
    c.sp.dma(lambda e: e.dma_start(out=gcol[:], in_=g_d[:, :]), gS, writes=[gcolB])
    ob = [c.sb(f"ob{i}", [128, TP], BF16) for i in range(2)]
    obB = [Buf(f"ob{i}") for i in range(2)]
    obS = [c.dsem(f"ob{i}") for i in range(2)]
    oi = [0]
    for ps_ in range(ntok // TP):
        t0 = ps_ * TP
        fin = xS.count + KD
        for m in range(KD):
            c.sp.dma(lambda e, m=m, t0=t0: e.dma_start(out=xT[:, m, :], in_=xT_d[m, :, t0:t0 + TP]), xS, writes=[xTB[m]], final=fin)
        tc.norm(xT, xTB, gcol, gcolB, 0, hT, hTB)

        def rhs_fn(kc, tb):
            return hT[:, kc, tb * 512:(tb + 1) * 512], [hTB[kc]]

        def evac(j, ps, psB, t0=t0):
            b = oi[0] % 2
            oi[0] += 1
            sc = (128 ** -0.5) if j < 8 else 1.0
            c.act.op(lambda e, b=b, sc=sc: e.activation(out=ob[b][:], in_=ps[:], func=AF.Copy, scale=sc),
                     reads=[psB], writes=[obB[b]])
            c.act.dma(lambda e, b=b, j=j: e.dma_start(out=out_d[j, :, t0:t0 + TP], in_=ob[b][:]), obS[b], reads=[obB[b]])
        tc.linear(w_d, KD, ncol_tiles, rhs_fn, evac)
        if dbg:
            dS = c.dsem("dbg")
            c.sp.dma(lambda e: e.dma_start(out=dbg_h[:, :, :], in_=hT[:]), dS, reads=hTB)
            c.sp.dma(lambda e: e.dma_start(out=dbg_r[:, :], in_=tc.rstd[:]), dS, reads=[tc.rstdB])
            c.sp.dma(lambda e: e.dma_start(out=dbg_w[0], in_=tc.wbf[0][:]), dS, reads=[tc.wbfB[0]])
            c.sp.dma(lambda e: e.dma_start(out=dbg_w[1], in_=tc.wbf[1][:]), dS, reads=[tc.wbfB[1]])
    return ctx.finish()


def build_phaseC(ntok, moe, final):
    ctx = Ctx()
    c = ctx
    NS = 8 if moe else 4
    xT_d = c.dram("xT", [16, 128, ntok], F32, "ExternalInput")
    g_d = c.dram("gcol", [128, 48], F32, "ExternalInput")
    oT_d = c.dram("oT", [8, 128, ntok], BF16, "ExternalInput")
    yT_d = c.dram("yT", [8, 128, ntok], BF16, "ExternalInput")
    wglu_d = c.dram("w_glu", [8, 128, 8 * 128], F32, "ExternalInput")
    pat_d = c.dram("p_attn", [16, 128, 8 * 128], F32, "ExternalInput")
    pss_d = c.dram("p_ssm", [16, 128, 8 * 128], F32, "ExternalInput")
    wg_d = c.dram("w_g", [32, 128, KD * 128], F32, "ExternalInput")
    wo_d = c.dram("w_out", [16, 128, KD * 128], F32, "ExternalInput")
    fg_d = c.dram("f_gate", [NS * 8, 128, KD * 128], F32, "ExternalInput")
    fu_d = c.dram("f_up", [NS * 8, 128, KD * 128], F32, "ExternalInput")
    fd_d = c.dram("f_down", [NS * 16, 128, 8 * 128], F32, "ExternalInput")
    if moe:
        wr_d = c.dram("w_router", [128, KD * 8], F32, "ExternalInput")
        id_d = c.dram("ident", [128, 128], BF16, "ExternalInput")
    out_d = c.dram("outT", [16, 128, ntok], F32, "ExternalOutput")
    tc = TokCommon(ctx, 3, 2, True) if moe else TokCommon(ctx)
    mul, add = ALU.mult, ALU.add
    XBUF = c.sb("XBUF", [128, KD, TP], F32)
    XB16 = XBUF[:].bitcast(BF16).rearrange("p (a t) -> p a t", t=TP) if False else XBUF[:].bitcast(BF16)
    def x16(i, lo=0, hi=TP):
        return XB16[:, i // 2, (i % 2) * TP + lo:(i % 2) * TP + hi]
    xTB = [Buf(f"xT{m}") for m in range(KD)]
    xS = c.dsem("xload")
    oyS = c.dsem("oyload")
    stS = [c.dsem("xst0"), c.dsem("xst1")]
    HB = c.sb("HBUF", [128, KD, TP], BF16)
    HBB = [Buf(f"hT{m}") for m in range(KD)]
    MB = c.sb("MBUF", [128, KD, TP], BF16)
    MBB = [Buf(f"mb{m}") for m in range(KD)]
    gcol = c.sb("gcol_s", [128, 48], F32)
    gcolB = Buf("gcol")
    gS = c.dsem("gload")
    c.sp.dma(lambda e: e.dma_start(out=gcol[:], in_=g_d[:, :]), gS, writes=[gcolB])
    S1 = c.sb("S1", [128, TP], F32); S1B = Buf("S1")
    S2 = c.sb("S2", [128, TP], F32); S2B = Buf("S2")
    T1 = c.sb("T1", [128, TP], F32); T1B = Buf("T1")
    T2 = c.sb("T2", [128, TP], F32); T2B = Buf("T2")
    sgb = c.sb("sgb", [128, TP], BF16); sgbB = Buf("sgb")
    if moe:
        wrs = c.sb("wr_st", [128, KD * 8], F32); wrb = c.sb("wr_bf", [128, KD * 8], BF16); wrB = Buf("wr")
        ident = c.sb("ident_s", [128, 128], BF16)
        c.sp.dma(lambda e: e.dma_start(out=wrs[:], in_=wr_d[:, :]), gS, writes=[wrB], final=3)
        c.sp.dma(lambda e: e.dma_start(out=ident[:], in_=id_d[:, :]), gS, writes=[wrB], final=3)
        gcolB.lw = (gS, 3)
        c.dve.op(lambda e: e.tensor_copy(out=wrb[:], in_=wrs[:]), reads=[wrB], writes=[wrB])
        lg = c.sb("lg", [128, 8, 8], F32); lgB = Buf("lg")
        m8 = c.sb("m8", [128, 8, 8], F32)
        gg = c.sb("gg", [128, 8, 2], F32)
        wts = c.sb("wts", [128, 8, 8], F32)
        wl = c.sb("wl", [128, 8, 128], BF16); wlB = Buf("wl")
        wbc = c.sb("wbc", [128, 8, TP], BF16); wbcB = Buf("wbc")

    for ps_ in range(ntok // TP):
        t0 = ps_ * TP

        def load_x(t0=t0):
            fin = xS.count + KD
            for m in range(KD):
                c.sp.dma(lambda e, m=m, t0=t0: e.dma_start(out=XBUF[:, m, :], in_=xT_d[m, :, t0:t0 + TP]), xS, writes=[xTB[m]], final=fin)
        load_x()
        tc.norm(XBUF, xTB, gcol, gcolB, 0, HB, HBB)
        fin = oyS.count + 16
        for i in range(8):
            c.sp.dma(lambda e, i=i, t0=t0: e.dma_start(out=x16(i), in_=oT_d[i, :, t0:t0 + TP]), oyS, writes=[xTB[i // 2]], final=fin)
            c.sp.dma(lambda e, i=i, t0=t0: e.dma_start(out=x16(8 + i), in_=yT_d[i, :, t0:t0 + TP]), oyS, writes=[xTB[(8 + i) // 2]], final=fin)

        def ev_glu(j, ps, psB):
            c.act.op(lambda e: e.activation(out=sgb[:], in_=ps[:], func=AF.Sigmoid), reads=[psB], writes=[sgbB])
            c.dve.op(lambda e: e.tensor_tensor(out=x16(16 + j), in0=x16(8 + j), in1=sgb[:], op=mul),
                     reads=[sgbB, xTB[(8 + j) // 2]], writes=[xTB[(16 + j) // 2]])
        tc.linear(wglu_d, 8, 8, lambda kc, tb: (x16(8 + kc, tb * 512, (tb + 1) * 512), [xTB[(8 + kc) // 2]]), ev_glu)
        rhs_h = lambda kc, tb: (HB[:, kc, tb * 512:(tb + 1) * 512], [HBB[kc]])
        for m in range(KD):
            def ev_ga(j, ps, psB):
                c.act.op(lambda e: e.activation(out=S1[:], in_=ps[:], func=AF.Sigmoid), reads=[psB], writes=[S1B])
            tc.linear(wg_d, KD, 1, rhs_h, ev_ga, j0=m)
            def ev_a(j, ps, psB):
                c.dve.op(lambda e: e.tensor_tensor(out=T1[:], in0=ps[:], in1=S1[:], op=mul), reads=[psB, S1B], writes=[T1B])
            tc.linear(pat_d, 8, 1, lambda kc, tb: (x16(kc, tb * 512, (tb + 1) * 512), [xTB[kc // 2]]), ev_a, j0=m)
            def ev_gb(j, ps, psB):
                c.act.op(lambda e: e.activation(out=S2[:], in_=ps[:], func=AF.Sigmoid), reads=[psB], writes=[S2B])
            tc.linear(wg_d, KD, 1, rhs_h, ev_gb, j0=16 + m)
            def ev_b(j, ps, psB, m=m):
                c.dve.op(lambda e: e.tensor_tensor(out=T2[:], in0=ps[:], in1=S2[:], op=mul), reads=[psB, S2B], writes=[T2B])
                c.pool.op(lambda e: e.tensor_tensor(out=MB[:, m, :], in0=T1[:], in1=T2[:], op=add), reads=[T1B, T2B], writes=[MBB[m]])
            tc.linear(pss_d, 8, 1, lambda kc, tb: (x16(16 + kc, tb * 512, (tb + 1) * 512), [xTB[(16 + kc) // 2]]), ev_b, j0=m)
        load_x()
        def ev_res(j, ps, psB):
            mm = j % 16
            c.dve.op(lambda e: e.tensor_tensor(out=XBUF[:, mm, :], in0=ps[:], in1=XBUF[:, mm, :], op=add), reads=[psB, xTB[mm]], writes=[xTB[mm]])
        tc.linear(wo_d, KD, 16, lambda kc, tb: (MB[:, kc, tb * 512:(tb + 1) * 512], [MBB[kc]]), ev_res)
        tc.norm(XBUF, xTB, gcol, gcolB, 1, HB, HBB)
        if moe:
            ps, psB = tc.next_pt()
            for tt in range(TP // 128):
                for kc in range(KD):
                    c.pe.op(lambda e, tt=tt, kc=kc, ps=ps: e.matmul(ps[:, tt * 8:(tt + 1) * 8], HB[:, kc, tt * 128:(tt + 1) * 128], wrb[:, kc * 8:(kc + 1) * 8],
                                                                  start=(kc == 0), stop=(kc == KD - 1)),
                            reads=[HBB[kc], wrB], writes=[psB], last=(kc == KD - 1))
            c.dve.op(lambda e, ps=ps: e.tensor_copy(out=lg[:], in_=ps[:, 0:64].rearrange("p (a b) -> p a b", b=8)), reads=[psB], writes=[lgB])
            for tt in range(TP // 128):
                c.dve.op(lambda e, tt=tt: e.max(out=m8[:, tt, :], in_=lg[:, tt, :]), reads=[lgB], writes=[lgB])
            c.dve.op(lambda e: e.tensor_tensor(out=gg[:, :, 0], in0=m8[:, :, 0], in1=m8[:, :, 1], op=ALU.subtract), reads=[lgB], writes=[lgB])
            c.act.op(lambda e: e.activation(out=gg[:, :, 0], in_=gg[:, :, 0], func=AF.Sigmoid), reads=[lgB], writes=[lgB])
            c.dve.op(lambda e: e.tensor_scalar(out=gg[:, :, 1], in0=gg[:, :, 0], scalar1=-1.0, scalar2=1.0, op0=mul, op1=add), reads=[lgB], writes=[lgB])
            for tt in range(TP // 128):
                c.dve.op(lambda e, tt=tt: e.tensor_scalar(out=wts[:, tt, :], in0=lg[:, tt, :], scalar1=m8[:, tt, 0:1], scalar2=gg[:, tt, 0:1],
                                                          op0=ALU.is_equal, op1=mul), reads=[lgB], writes=[lgB])
                c.dve.op(lambda e, tt=tt: e.tensor_scalar(out=m8[:, tt, :], in0=lg[:, tt, :], scalar1=m8[:, tt, 1:2], scalar2=gg[:, tt, 1:2],
                                                          op0=ALU.is_equal, op1=mul), reads=[lgB], writes=[lgB])
                c.dve.op(lambda e, tt=tt: e.tensor_tensor(out=wts[:, tt, :], in0=wts[:, tt, :], in1=m8[:, tt, :], op=add), reads=[lgB], writes=[lgB])
            for tt in range(TP // 128):
                c.dve.op(lambda e, tt=tt: e.tensor_copy(out=wl[:], in_=wts[:, tt, :].unsqueeze(2).to_broadcast([128, 8, 128])), reads=[lgB], writes=[wlB])
                for e_ in range(8):
                    pw, pwB = tc.pt[(tc.pti + e_ // 2) % 4], tc.ptB[(tc.pti + e_ // 2) % 4]
                ps2, ps2B = tc.next_pt()
                for e_ in range(8):
                    c.pe.op(lambda e, e_=e_, ps2=ps2: e.matmul(ps2[:, e_ * 128:(e_ + 1) * 128], wl[:, e_, :], ident[:], start=True, stop=True),
                            reads=[wlB, wrB], writes=[ps2B], last=(e_ == 7))
                c.act.op(lambda e, tt=tt, ps2=ps2: e.activation(out=wbc[:, :, tt * 128:(tt + 1) * 128], in_=ps2[:, 0:1024].rearrange("p (a b) -> p a b", b=128), func=AF.Copy),
                         reads=[ps2B], writes=[wbcB])
        for s in range(NS):
            for f in range(8):
                def ev_g(j, ps, psB):
                    c.act.op(lambda e: e.activation(out=S1[:], in_=ps[:], func=AF.Silu), reads=[psB], writes=[S1B])
                tc.linear(fg_d, KD, 1, rhs_h, ev_g, j0=s * 8 + f)
                def ev_u(j, ps, psB, f=f, s=s):
                    if moe:
                        c.dve.op(lambda e: e.tensor_tensor(out=T1[:], in0=ps[:], in1=S1[:], op=mul), reads=[psB, S1B], writes=[T1B])
                        c.pool.op(lambda e: e.tensor_tensor(out=MB[:, f, :], in0=T1[:], in1=wbc[:, s, :], op=mul), reads=[T1B, wbcB], writes=[MBB[f]])
                    else:
                        c.dve.op(lambda e: e.tensor_tensor(out=MB[:, f, :], in0=ps[:], in1=S1[:], op=mul), reads=[psB, S1B], writes=[MBB[f]])
                tc.linear(fu_d, KD, 1, rhs_h, ev_u, j0=s * 8 + f)
            tc.linear(fd_d, 8, 16, lambda kc, tb: (MB[:, kc, tb * 512:(tb + 1) * 512], [MBB[kc]]), ev_res, j0=s * 16)
        if final:
            tc.norm_stats(XBUF, xTB)
        for m in range(KD):
            if final:
                b = m % 2
                tb_, tbB = (T1, T1B) if b == 0 else (T2, T2B)
                c.dve.op(lambda e, m=m, tb_=tb_: e.scalar_tensor_tensor(out=tb_[:], in0=XBUF[:, m, :], scalar=gcol[:, 32 + m:33 + m], in1=tc.rstd[:], op0=mul, op1=mul),
                         reads=[xTB[m], gcolB, tc.rstdB], writes=[tbB])
                c.sp.dma(lambda e, m=m, tb_=tb_, t0=t0: e.dma_start(out=out_d[m, :, t0:t0 + TP], in_=tb_[:]), stS[b], reads=[tbB])
            else:
                c.sp.dma(lambda e, m=m, t0=t0: e.dma_start(out=out_d[m, :, t0:t0 + TP], in_=XBUF[:, m, :]), stS[m % 2], reads=[xTB[m]], final=stS[m % 2].count + (KD - m + 1) // 2)
    return ctx.finish()

S = 16384
NQB = S // 512


def attn_consts():
    i = np.arange(128)
    ntri = -(i[:, None] >= i[None, :]).astype(np.float32)
    mask = (i[:, None] < i[None, :]).astype(np.float32)
    arrs = [np.eye(128, dtype=np.float32), ntri, -np.ones((128, 128), np.float32), mask, np.zeros((128, 128), np.float32)]
    return {"cpack": np.ascontiguousarray(np.stack(arrs, axis=1)).astype(NPBF)}


def emit_attention(ctx, qT_d, kT_d, vT_d, oT_d, pss, tps, nqb=NQB, stages=5, alias=None, tick=None):
    c = ctx
    nk = nqb * 4
    L = nqb * 512
    qT = c.sb("qT_s", [128, L], BF16)
    kT = c.sb("kT_s", [128, L], BF16)
    if alias is None:
        vtok = c.sb("vtok_s", [128, nk, 128], BF16)
        aB = []
    else:
        vtok = alias[0][:].rearrange("p (a b) -> p a b", b=128)
        aB = [alias[1]]
    qB, kB, vTB = Buf("qT"), Buf("kT"), Buf("vT")
    vtokB = [Buf(f"vtok{i}") for i in range(nk)]
    ldS = c.dsem("attn_ld")
    cB = Buf("attn_consts")
    cS = c.dsem("attn_c")
    cd = c.dram("cpack", [128, 5, 128], BF16, "ExternalInput")
    cp = c.sb("cpack_s", [128, 5, 128], BF16)
    c.sp.dma(lambda e: e.dma_start(out=cp[:], in_=cd[:, :, :]), cS, writes=[cB])
    ident, ntri, nones, mask, zeros = [cp[:, i, :] for i in range(5)]
    nch = max(1, L // 4096)
    cw = L // nch
    for (sb_, d_, B_, nm_) in [(qT, qT_d, qB, "attn_q"), (kT, kT_d, kB, "attn_k")]:
        gS_ = c.dsem(nm_)
        for i in range(nch):
            c.sp.dma(lambda e, sb_=sb_, d_=d_, i=i: e.dma_start(out=sb_[:, i * cw:(i + 1) * cw], in_=d_[:, i * cw:(i + 1) * cw]),
                     gS_, writes=[B_], final=nch)
    fin = nch
    tw = nk // nch
    for i in range(nch):
        c.sp.dma(lambda e, i=i: e.dma_start(out=vtok[:, i * tw:(i + 1) * tw, :], in_=vT_d[:, i * tw:(i + 1) * tw, :]),
                 ldS, writes=vtokB[i * tw:(i + 1) * tw] + aB, final=fin)
    NST = 2
    eb = [c.sb(f"eb{i}", [128, 512], F32) for i in range(NST)]
    ebB = [Buf(f"eb{i}") for i in range(NST)]
    spb = [c.sb(f"spb{i}", [128, 512], BF16) for i in range(NST)]
    spB = [Buf(f"spb{i}") for i in range(NST)]
    wb = [c.sb(f"wb{i}", [128, 512], BF16) for i in range(NST)]
    wbB = [Buf(f"wb{i}") for i in range(NST)]
    acc = [c.sb(f"acc{i}", [128, 512], BF16) for i in range(2)]
    accB = [Buf(f"acc{i}") for i in range(2)]
    osb = [c.sb(f"osb{i}", [128, 512], BF16) for i in range(2)]
    osbB = [Buf(f"osb{i}") for i in range(2)]
    osS = [c.dsem(f"osb{i}") for i in range(2)]
    zps = pss[0:2]
    wps = pss[2:4]
    ops = pss[4:5]

    items = []
    for i in range(nqb):
        for p in range(4 * i + 3, -1, -1):
            jj = p - 4 * i
            c0 = 128 * jj if jj >= 0 else 0
            items.append((i, p, c0, jj >= 0, p == 4 * i + 3, p == 0))
    N = len(items)

    def st1(n):
        i, p, c0, diag, first, lastp = items[n]
        ps, psB = zps[n % 2]
        t0 = i * 512
        c.pe.op(lambda e, ps=ps, p=p, c0=c0, t0=t0: e.matmul(ps[:, c0:512], kT[:, p * 128:(p + 1) * 128], qT[:, t0 + c0:t0 + 512],
                                                             start=True, stop=True),
                reads=[kB, qB], writes=[psB])

    def st2(n):
        i, p, c0, diag, first, lastp = items[n]
        ps, psB = zps[n % 2]
        s = n % NST
        c.act.op(lambda e, ps=ps, s=s, c0=c0: e.activation(out=eb[s][:, c0:512], in_=ps[:, c0:512], func=AF.Exp),
                 reads=[psB], writes=[ebB[s]])
        c.act.op(lambda e, s=s, c0=c0: e.activation(out=spb[s][:, c0:512], in_=eb[s][:, c0:512], func=AF.Ln, bias=1.0),
                 reads=[ebB[s]], writes=[spB[s]])
        if diag:
            c.pool.op(lambda e, s=s, c0=c0: e.tensor_tensor(out=spb[s][:, c0:c0 + 128], in0=spb[s][:, c0:c0 + 128], in1=mask, op=ALU.mult),
                      reads=[spB[s], cB], writes=[spB[s]])

    def st3(n):
        i, p, c0, diag, first, lastp = items[n]
        ps, psB = wps[n % 2]
        s = n % NST
        a = i % 2
        t0 = i * 512
        c.pe.op(lambda e, ps=ps, p=p, c0=c0, t0=t0: e.matmul(ps[:, c0:512], kT[:, p * 128:(p + 1) * 128], qT[:, t0 + c0:t0 + 512],
                                                             start=True, stop=False),
                reads=[kB, qB], writes=[psB], last=False)
        c.pe.op(lambda e, ps=ps, s=s, c0=c0: e.matmul(ps[:, c0:512], ntri, spb[s][:, c0:512], start=False, stop=first),
                reads=[cB, spB[s]], writes=[psB], last=first)
        if not first:
            c.pe.op(lambda e, ps=ps, a=a, c0=c0: e.matmul(ps[:, c0:512], nones, acc[a][:, c0:512], start=False, stop=True),
                    reads=[cB, accB[a]], writes=[psB])

    def st4(n):
        i, p, c0, diag, first, lastp = items[n]
        ps, psB = wps[n % 2]
        s = n % NST
        a = i % 2
        c.act.op(lambda e, ps=ps, s=s, c0=c0: e.activation(out=wb[s][:, c0:512], in_=ps[:, c0:512], func=AF.Exp),
                 reads=[psB], writes=[wbB[s]])
        if diag:
            c.pool.op(lambda e, s=s, c0=c0: e.tensor_tensor(out=wb[s][:, c0:c0 + 128], in0=wb[s][:, c0:c0 + 128], in1=mask, op=ALU.mult),
                      reads=[wbB[s], cB], writes=[wbB[s]])
        if not lastp:
            if first:
                c.pool.op(lambda e, a=a: e.memset(acc[a][:], 0.0), writes=[accB[a]])
            c.pool.op(lambda e, a=a, s=s, c0=c0: e.tensor_tensor(out=acc[a][:, c0:512], in0=acc[a][:, c0:512], in1=spb[s][:, c0:512], op=ALU.add),
                      reads=[accB[a], spB[s]], writes=[accB[a]])

    def st5(n):
        i, p, c0, diag, first, lastp = items[n]
        ps, psB = ops[0]
        s = n % NST
        if first:
            c.pe.op(lambda e, ps=ps: e.matmul(ps[:, 0:512], zeros, qT[:, 0:512], start=True, stop=False),
                    reads=[cB, qB], writes=[psB], last=False)
        c.pe.op(lambda e, ps=ps, p=p, s=s, c0=c0: e.matmul(ps[:, c0:512], vtok[:, p, :], wb[s][:, c0:512], start=False, stop=lastp),
                reads=[vtokB[p], wbB[s]], writes=[psB])
        if lastp:
            o = i % 2
            t0 = i * 512
            c.act.op(lambda e, ps=ps, o=o: e.activation(out=osb[o][:], in_=ps[:, 0:512], func=AF.Copy), reads=[psB], writes=[osbB[o]])
            c.act.dma(lambda e, o=o, t0=t0: e.dma_start(out=oT_d[:, t0:t0 + 512], in_=osb[o][:]), osS[o], reads=[osbB[o]])

    if stages == -1:
        return
    if stages == 0:
        c.sp.dma(lambda e: e.dma_start(out=oT_d[:, 0:512], in_=vtok[:, 0:4, :].rearrange('p a b -> p (a b)')), osS[0], reads=vtokB)
        return
    for n in range(N + 3):
        if tick is not None:
            tick(n / float(N))
        if n < N and stages >= 1:
            st1(n)
        if 0 <= n - 1 < N and stages >= 2:
            st2(n - 1)
        if 0 <= n - 2 < N and stages >= 3:
            st3(n - 2)
            if stages >= 4:
                st4(n - 2)
        if 0 <= n - 3 < N and stages >= 5:
            st5(n - 3)

S = 16384
BW = 512
NSTEP = 9
PKW = 12 + 128 + 1 + 192 + 128 + 512 + 512


def ssm_pack(a_re, a_im, log_dt, b_re, b_im, c_re, c_im, dsk, h):
    G = slice(8 * h, 8 * h + 8)
    are, aim, ldt = a_re[G], a_im[G], log_dt[G]
    bre, bim, cre, cim, dd = b_re[G], b_im[G], c_re[G], c_im[G], dsk[G]
    l1 = lambda t: t.reshape(4, 2, 64).transpose(1, 2, 0).reshape(128, 4)
    small = np.stack([l1(are), l1(aim), l1(np.repeat(ldt[:, None], 64, 1))], axis=2).reshape(128, 12)
    c1 = lambda t: np.tile(t.transpose(0, 2, 1).reshape(4, 2, 64, 16).transpose(1, 2, 0, 3).reshape(128, 4, 1, 16), (1, 1, 8, 1)).reshape(128, 4 * 128)
    dcol = dd.reshape(128, 1)
    rep = lambda t: np.repeat(t, 16, axis=0)
    p2 = np.concatenate([rep(are), rep(aim), rep(np.repeat(ldt[:, None], 64, 1))], axis=1)
    b2 = lambda t: t.transpose(0, 2, 1).reshape(128, 64)
    ch = np.arange(128)
    r = np.arange(128)
    m1 = np.zeros((128, 4, 128), np.float32)
    m2 = np.zeros((128, 4, 128), np.float32)
    for m in range(4):
        for g2 in range(2):
            rows = (ch // 32 == m) & ((ch // 16) % 2 == g2)
            m1[np.ix_(rows, [m], np.arange(64 * g2, 64 * g2 + 64))] = 1.0
            rr = (r // 64 == g2)
            cols = (ch // 16 == 2 * m + g2)
            m2[np.ix_(rr, [m], np.nonzero(cols)[0])] = 1.0
    pk = np.concatenate([small, c1(cre)[:, :0], dcol * 0 + 0, ], axis=1) if False else None
    pack = np.concatenate([small, np.concatenate([b2(bre), b2(bim)], axis=1), dcol, p2,
                           np.zeros((128, 128), np.float32), m1.reshape(128, 512), m2.reshape(128, 512)], axis=1).astype(np.float32)
    cpk = np.concatenate([c1(cre), c1(cim)], axis=1).astype(np.float32)
    assert pack.shape[1] == PKW, pack.shape
    return pack, cpk


class T:
    def __init__(self, ctx):
        self.c = ctx
        self.n = 0
        self.free = {}

    def new(self, shape):
        key = tuple(int(x) for x in shape)
        if self.free.get(key):
            return self.free[key].pop()
        self.n += 1
        return (self.c.sb(f"sst{self.n}", list(key), F32), Buf(f"sst{self.n}"))

    def rel(self, *tiles):
        for x in tiles:
            if hasattr(x[0], "name"):
                self.free.setdefault(tuple(int(v) for v in x[0].shape), []).append(x)

    def tt(self, a, b, op, shape=None, out=None):
        o = out or self.new(shape or list(a[0].shape))
        self.c.dve.op(lambda e: e.tensor_tensor(out=o[0][:], in0=a[0][:], in1=b[0][:], op=op), reads=[a[1], b[1]], writes=[o[1]])
        return o

    def ts(self, a, s1, op0, s2=None, op1=None, out=None):
        o = out or self.new(list(a[0].shape))
        if op1 is None:
            self.c.dve.op(lambda e: e.tensor_scalar(out=o[0][:], in0=a[0][:], scalar1=s1, scalar2=None, op0=op0), reads=[a[1]], writes=[o[1]])
        else:
            self.c.dve.op(lambda e: e.tensor_scalar(out=o[0][:], in0=a[0][:], scalar1=s1, scalar2=s2, op0=op0, op1=op1), reads=[a[1]], writes=[o[1]])
        return o

    def act(self, a, func, scale=1.0, out=None):
        o = out or self.new(list(a[0].shape))
        self.c.act.op(lambda e: e.activation(out=o[0][:], in_=a[0][:], func=func, scale=scale), reads=[a[1]], writes=[o[1]])
        return o

    def recip(self, a):
        o = self.new(list(a[0].shape))
        self.c.dve.op(lambda e: e.reciprocal(out=o[0][:], in_=a[0][:]), reads=[a[1]], writes=[o[1]])
        return o


def ssm_params(t, are, aim, ldt, want_z):
    mul, add, sub = ALU.mult, ALU.add, ALU.subtract
    dtv = t.act(ldt, AF.Exp)
    tre = t.tt(dtv, are, mul)
    ang = t.tt(dtv, aim, mul)
    t.rel(dtv)
    mag = t.act(tre, AF.Exp)
    t.rel(tre)
    s = t.act(ang, AF.Sin, scale=1.0 / 64)
    sh = t.act(ang, AF.Sin, scale=1.0 / 128)
    t.rel(ang)
    sh2 = t.tt(sh, sh, mul)
    c = t.ts(sh2, -2.0, mul, 1.0, add)
    t.rel(sh, sh2)
    for _ in range(6):
        sc = t.tt(s, c, mul)
        cc = t.tt(c, c, mul)
        ss = t.tt(s, s, mul)
        t.rel(s, c)
        s = t.ts(sc, 2.0, mul)
        c = t.tt(cc, ss, sub)
        t.rel(sc, cc, ss)
    abr = t.tt(mag, c, mul)
    abi = t.tt(mag, s, mul)
    t.rel(mag, c, s)
    if not want_z:
        return abr, abi, None, None
    a2 = t.tt(are, are, mul)
    b2 = t.tt(aim, aim, mul)
    den = t.tt(a2, b2, add)
    rden = t.recip(den)
    t.rel(a2, b2, den)
    nre = t.ts(abr, -1.0, add)
    p1 = t.tt(nre, are, mul)
    p2 = t.tt(abi, aim, mul)
    p3 = t.tt(p1, p2, add)
    zr = t.tt(p3, rden, mul)
    t.rel(p1, p2, p3)
    p1 = t.tt(abi, are, mul)
    p2 = t.tt(nre, aim, mul)
    p3 = t.tt(p1, p2, sub)
    zi = t.tt(p3, rden, mul)
    t.rel(p1, p2, p3, rden, nre)
    return abr, abi, zr, zi


def emit_ssm(ctx, uT_d, pack_d, cpk_d, yT_d, pss, nblk=S // BW):
    c = ctx
    t = T(ctx)
    mul, add, sub = ALU.mult, ALU.add, ALU.subtract
    L = nblk * BW
    pk = c.sb("ssm_pk", [128, PKW], F32)
    pkB = Buf("ssm_pk")
    pS = c.dsem("ssm_pk")
    c.sp.dma(lambda e: e.dma_start(out=pk[:], in_=pack_d[:, :]), pS, writes=[pkB])
    cpk = c.sb("ssm_cpk", [128, 1024], F32)
    cpkB = Buf("ssm_cpk")
    c.sp.dma(lambda e: e.dma_start(out=cpk[:], in_=cpk_d[:, :]), pS, writes=[cpkB], final=2)
    pkB.lw = (pS, 2)
    uT = c.sb("uT_s", [128, L], BF16)
    uB = Buf("uT")
    uS = c.dsem("ssm_u")
    nch = max(1, L // 4096)
    cw = L // nch
    for i in range(nch):
        c.sp.dma(lambda e, i=i: e.dma_start(out=uT[:, i * cw:(i + 1) * cw], in_=uT_d[:, i * cw:(i + 1) * cw]), uS, writes=[uB], final=nch)

    class V:
        def __init__(self, ap):
            self.ap = ap

        def __getitem__(self, k):
            return self.ap

        @property
        def shape(self):
            return self.ap.shape
    small = pk[:, 0:12].rearrange("p (m k) -> p m k", k=3)
    are1, aim1, ldt1 = [(V(small[:, :, k]), pkB) for k in range(3)]
    b2r = (V(pk[:, 12:76]), pkB)
    b2i = (V(pk[:, 76:140]), pkB)
    dcol = pk[:, 140:141]
    are2, aim2, ldt2 = [(V(pk[:, 141 + 64 * k:141 + 64 * (k + 1)]), pkB) for k in range(3)]
    m1 = pk[:, 461:973].rearrange("p (m k) -> p m k", k=128)
    m2 = pk[:, 973:1485].rearrange("p (m k) -> p m k", k=128)

    abr, abi, _, _ = ssm_params(t, are1, aim1, ldt1, False)
    pw_r = [abr]
    pw_i = [abi]
    for k in range(1, NSTEP):
        r_, i_ = pw_r[-1], pw_i[-1]
        rr = t.tt(r_, r_, mul)
        ii = t.tt(i_, i_, mul)
        ri = t.tt(r_, i_, mul)
        pw_r.append(t.tt(rr, ii, sub))
        pw_i.append(t.ts(ri, 2.0, mul))
        t.rel(rr, ii, ri)
    npw_i = [t.ts(x, -1.0, mul) for x in pw_i]
    _, _, zr2, zi2 = ssm_params(t, are2, aim2, ldt2, True)
    q1 = t.tt(zr2, b2r, mul)
    q2 = t.tt(zi2, b2i, mul)
    bbr = t.tt(q1, q2, sub)
    t.rel(q1, q2)
    q1 = t.tt(zr2, b2i, mul)
    q2 = t.tt(zi2, b2r, mul)
    bbi = t.tt(q1, q2, add)
    t.rel(q1, q2)
    BT = c.sb("ssm_BT", [128, 4, 2, 128], BF16)
    BTB = Buf("ssm_BT")
    for m in range(4):
        for ri, src in enumerate([bbr, bbi]):
            c.dve.op(lambda e, m=m, ri=ri, src=src: e.tensor_tensor(
                out=BT[:, m, ri, :].rearrange("p (a n) -> p a n", a=2), in0=m1[:, m, :].rearrange("p (a n) -> p a n", a=2),
                in1=src[0][:].unsqueeze(1).to_broadcast([128, 2, 64]), op=mul),
                reads=[pkB, src[1]], writes=[BTB])
    ZC = c.sb("ssm_ZC", [128, 4, 2, 128], BF16)
    ZCB = Buf("ssm_ZC")
    for m in range(4):
        c.dve.op(lambda e, m=m: e.tensor_tensor(out=ZC[:, m, 0, :], in0=m2[:, m, :], in1=cpk[:, m * 128:(m + 1) * 128], op=mul),
                 reads=[pkB, cpkB], writes=[ZCB])
        c.dve.op(lambda e, m=m: e.scalar_tensor_tensor(out=ZC[:, m, 1, :], in0=m2[:, m, :], scalar=-1.0, in1=cpk[:, 512 + m * 128:512 + (m + 1) * 128],
                                                       op0=mul, op1=mul),
                 reads=[pkB, cpkB], writes=[ZCB])

    XS = [[c.sb(f"ssm_X{p}{q}", [128, 2, BW], F32) for q in range(2)] for p in range(2)]
    XSB = [[[Buf(f"X{p}{q}{k}") for k in range(2)] for q in range(2)] for p in range(2)]
    XH = c.sb("ssm_XH", [128, 4, 2, BW], BF16)
    XHB = [Buf(f"XH{m}") for m in range(4)]
    CAR = c.sb("ssm_car", [128, 4, 4], F32)
    CARB = [Buf(f"car{m}") for m in range(4)]

    def hs_scan(ms, slots):
        def level(k, o0, i0_, st, cnt):
            first, second = [], []
            for m, sl in zip(ms, slots):
                X = XS[sl][0]
                XB_ = XSB[sl][0]
                pr = pw_r[k][0][:][:, m:m + 1]
                pi = pw_i[k][0][:][:, m:m + 1]
                npi = npw_i[k][0][:][:, m:m + 1]
                pB = [pw_r[k][1], pw_i[k][1], npw_i[k][1]]
                osl = slice(o0, o0 + (cnt - 1) * st + 1, st)
                isl = slice(i0_, i0_ + (cnt - 1) * st + 1, st)
                first.append((lambda e, X=X, osl=osl, isl=isl, pr=pr: e.scalar_tensor_tensor(
                    out=X[:, 0, osl], in0=X[:, 0, isl], scalar=pr, in1=X[:, 0, osl], op0=mul, op1=add), [XB_[0]] + pB, [XB_[0]]))
                first.append((lambda e, X=X, osl=osl, isl=isl, pr=pr: e.scalar_tensor_tensor(
                    out=X[:, 1, osl], in0=X[:, 1, isl], scalar=pr, in1=X[:, 1, osl], op0=mul, op1=add), [XB_[1]] + pB, [XB_[1]]))
                second.append((lambda e, X=X, osl=osl, isl=isl, npi=npi: e.scalar_tensor_tensor(
                    out=X[:, 0, osl], in0=X[:, 1, isl], scalar=npi, in1=X[:, 0, osl], op0=mul, op1=add), [XB_[0], XB_[1]] + pB, [XB_[0]]))
                second.append((lambda e, X=X, osl=osl, isl=isl, pi=pi: e.scalar_tensor_tensor(
                    out=X[:, 1, osl], in0=X[:, 0, isl], scalar=pi, in1=X[:, 1, osl], op0=mul, op1=add), [XB_[0], XB_[1]] + pB, [XB_[1]]))
            for fn, rd, wr in first + second:
                c.dve.op(fn, reads=rd, writes=wr)
        for k in range(NSTEP):
            h = 1 << k
            st = h << 1
            level(k, st - 1, h - 1, st, BW // st)
        for k in range(NSTEP - 2, -1, -1):
            h = 1 << k
            st = h << 1
            cnt = BW // st - 1
            if cnt > 0:
                level(k, st + h - 1, st - 1, st, cnt)
        return [(XS[sl][0], XSB[sl][0]) for sl in slots]

    ypre = c.sb("ssm_ypre", [128, BW], F32)
    ypB = Buf("ypre")
    g1 = c.sb("ssm_g1", [128, BW], F32)
    g1B = Buf("g1")
    yo = [c.sb(f"ssm_yo{i}", [128, BW], BF16) for i in range(2)]
    yoB = [Buf(f"yo{i}") for i in range(2)]
    yoS = [c.dsem(f"ssm_yo{i}") for i in range(2)]

    def stage_in(blk):
            t0 = blk * BW
            for pair in range(2):
                ms = [2 * pair, 2 * pair + 1]
                for sl, m in enumerate(ms):
                    psr, psrB = pss[0]
                    psi, psiB = pss[1]
                    c.pe.op(lambda e, m=m, psr=psr, t0=t0: e.matmul(psr[:, 0:BW], BT[:, m, 0, :], uT[:, t0:t0 + BW], start=True, stop=True),
                            reads=[BTB, uB], writes=[psrB])
                    c.pe.op(lambda e, m=m, psi=psi, t0=t0: e.matmul(psi[:, 0:BW], BT[:, m, 1, :], uT[:, t0:t0 + BW], start=True, stop=True),
                            reads=[BTB, uB], writes=[psiB])
                    c.dve.op(lambda e, psr=psr, sl=sl: e.tensor_copy(out=XS[sl][0][:, 0, :], in_=psr[:, 0:BW]), reads=[psrB], writes=[XSB[sl][0][0]])
                    c.dve.op(lambda e, psi=psi, sl=sl: e.tensor_copy(out=XS[sl][0][:, 1, :], in_=psi[:, 0:BW]), reads=[psiB], writes=[XSB[sl][0][1]])
                if blk > 0:
                    for sl, m in enumerate(ms):
                        ar_ = abr[0][:][:, m:m + 1]
                        ai_ = abi[0][:][:, m:m + 1]
                        for (dst, src_, sc_) in [(0, 0, ar_), (0, 2, ai_), (1, 1, ar_), (1, 0, ai_)]:
                            c.dve.op(lambda e, sl=sl, dst=dst, src_=src_, sc_=sc_, m=m: e.scalar_tensor_tensor(
                                out=XS[sl][0][:, dst, 0:1], in0=CAR[:, m, src_:src_ + 1], scalar=sc_, in1=XS[sl][0][:, dst, 0:1], op0=mul, op1=add),
                                reads=[XSB[sl][0][dst], CARB[m], abr[1], abi[1]], writes=[XSB[sl][0][dst]])
                res = hs_scan(ms, [0, 1])
                for (r_, rB), m in zip(res, ms):
                    c.dve.op(lambda e, r_=r_, m=m: e.tensor_copy(out=CAR[:, m, 0:2], in_=r_[:, :, BW - 1]), reads=rB, writes=[CARB[m]])
                    c.dve.op(lambda e, r_=r_, m=m: e.tensor_scalar(out=CAR[:, m, 2:3], in0=r_[:, 1, BW - 1:BW], scalar1=-1.0, scalar2=None, op0=mul),
                             reads=rB, writes=[CARB[m]])
                    c.dve.op(lambda e, r_=r_, m=m: e.tensor_copy(out=XH[:, m, :, :], in_=r_[:]), reads=rB, writes=[XHB[m]])
    def stage_out(blk):
            t0 = blk * BW
            py, pyB = pss[2]
            for m in range(4):
                for ri in range(2):
                    c.pe.op(lambda e, py=py, m=m, ri=ri: e.matmul(py[:, 0:BW], ZC[:, m, ri, :], XH[:, m, ri, :], start=(m == 0 and ri == 0), stop=(m == 3 and ri == 1)),
                            reads=[ZCB, XHB[m]], writes=[pyB], last=(m == 3 and ri == 1))
            c.dve.op(lambda e, py=py, t0=t0: e.scalar_tensor_tensor(out=ypre[:], in0=uT[:, t0:t0 + BW], scalar=dcol, in1=py[:, 0:BW], op0=mul, op1=add),
                     reads=[uB, pkB, pyB], writes=[ypB])
            c.pool.op(lambda e: e.tensor_tensor(out=g1[:], in0=ypre[:], in1=ypre[:], op=mul), reads=[ypB], writes=[g1B])
            c.pool.op(lambda e: e.tensor_scalar(out=g1[:], in0=g1[:], scalar1=0.044715, scalar2=1.0, op0=mul, op1=add), reads=[g1B], writes=[g1B])
            c.pool.op(lambda e: e.tensor_tensor(out=g1[:], in0=g1[:], in1=ypre[:], op=mul), reads=[g1B, ypB], writes=[g1B])
            c.act.op(lambda e: e.activation(out=g1[:], in_=g1[:], func=AF.Sigmoid, scale=1.5957691216057308), reads=[g1B], writes=[g1B])
            o_ = blk % 2
            c.pool.op(lambda e, o_=o_: e.tensor_tensor(out=yo[o_][:], in0=g1[:], in1=ypre[:], op=mul), reads=[g1B, ypB], writes=[yoB[o_]])
            c.pool.dma(lambda e, o_=o_, t0=t0: e.dma_start(out=yT_d[:, t0:t0 + BW], in_=yo[o_][:]), yoS[o_], reads=[yoB[o_]])

    def blocks():
        for blk in range(nblk):
            if blk > 0:
                stage_out(blk - 1)
            stage_in(blk)
            yield blk
        stage_out(nblk - 1)
    return blocks()


def build_phaseB(L=S):
    ctx = Ctx()
    c = ctx
    qd = c.dram("qT", [128, L], BF16, "ExternalInput")
    kd = c.dram("kT", [128, L], BF16, "ExternalInput")
    vd = c.dram("vtok", [128, L // 128, 128], BF16, "ExternalInput")
    ud = c.dram("uT", [128, L], BF16, "ExternalInput")
    pd = c.dram("spack", [128, PKW], F32, "ExternalInput")
    cd = c.dram("scpk", [128, 1024], F32, "ExternalInput")
    od = c.dram("oT", [128, L], BF16, "ExternalOutput")
    yd = c.dram("yT", [128, L], BF16, "ExternalOutput")
    pss = [(c.ps(f"pb{i}", [128, 512])[:], Buf(f"pb{i}")) for i in range(8)]
    nb = L // BW
    gen = emit_ssm(ctx, ud, pd, cd, yd, pss[5:8], nblk=nb)
    done = [0]

    def tick(frac):
        while done[0] < nb and done[0] < frac * nb + 1:
            next(gen)
            done[0] += 1
    emit_attention(ctx, qd, kd, vd, od, pss[:5], None, nqb=L // 512, tick=tick)
    for _ in gen:
        pass
    return ctx.finish()


_PROGS = {}


def _prog(key, fn):
    if key not in _PROGS:
        _PROGS[key] = fn()
    return _PROGS[key]


def _run(nc, in_maps):
    res = run_bass_kernel_spmd(nc, in_maps, core_ids=list(range(8)))
    return res.results


def kernel(**inp):
    NCORE = 8
    TOK = S // NCORE
    f32 = np.float32
    x = np.asarray(inp['x'], f32)[0]
    xT = [to_xT(x[c * TOK:(c + 1) * TOK]) for c in range(NCORE)]
    consts = attn_consts()
    ident = np.eye(128, dtype=f32).astype(NPBF)
    for layer in range(2):
        w_in = np.asarray(inp['w_in'][layer], f32)
        gA = gcols([np.asarray(inp['mix_norm'][layer], f32)])
        wq = wtiles(w_in[:, :4096])
        ncA = _prog('A', lambda: build_phaseA(TOK, 32))
        rA = _run(ncA, [{"xT": xT[c], "gcol": gA, "w_qkvu": wq} for c in range(NCORE)])
        P = [np.concatenate([rA[c]['projT'][j] for c in range(NCORE)], axis=1) for j in range(32)]
        del rA
        ncB = _prog('B', build_phaseB)
        mapsB = []
        for h in range(NCORE):
            pack, cpk = ssm_pack(*[np.asarray(inp[k][layer], f32) for k in
                                   ['ssm_a_re', 'ssm_a_im', 'ssm_log_dt', 'ssm_b_re', 'ssm_b_im', 'ssm_c_re', 'ssm_c_im', 'ssm_d']], h)
            vt = np.ascontiguousarray(P[16 + h].T.reshape(S // 128, 128, 128).transpose(1, 0, 2))
            mapsB.append({"qT": P[h], "kT": P[8 + h], "vtok": vt, "uT": P[24 + h], "spack": pack, "scpk": cpk, "cpack": consts["cpack"]})
        rB = _run(ncB, mapsB)
        del P, mapsB
        moe = (layer % 2 == 1)
        final = (layer == 1)
        gC = gcols([np.asarray(inp['mix_norm'][layer], f32), np.asarray(inp['ffn_norm'][layer], f32), np.asarray(inp['final_norm'], f32)])
        wC = {
            "gcol": gC,
            "w_glu": wtiles(np.asarray(inp['w_glu'][layer], f32)),
            "p_attn": wtiles(np.asarray(inp['p_attn'][layer], f32)),
            "p_ssm": wtiles(np.asarray(inp['p_ssm'][layer], f32)),
            "w_g": wtiles(w_in[:, 4096:]),
            "w_out": wtiles(np.asarray(inp['w_out'][layer], f32)),
        }
        i = layer // 2
        if not moe:
            wd = np.asarray(inp['ffn_w_down'][i], f32)
            wC["f_gate"] = wtiles(np.asarray(inp['ffn_w_gate'][i], f32))
            wC["f_up"] = wtiles(np.asarray(inp['ffn_w_up'][i], f32))
            wC["f_down"] = np.concatenate([wtiles(wd[1024 * s:1024 * (s + 1)]) for s in range(4)], axis=0)
        else:
            mg = np.asarray(inp['moe_w_gate'][i], f32)
            mu = np.asarray(inp['moe_w_up'][i], f32)
            md = np.asarray(inp['moe_w_down'][i], f32)
            wC["f_gate"] = np.concatenate([wtiles(mg[e]) for e in range(8)], axis=0)
            wC["f_up"] = np.concatenate([wtiles(mu[e]) for e in range(8)], axis=0)
            wC["f_down"] = np.concatenate([wtiles(md[e]) for e in range(8)], axis=0)
            wr = np.asarray(inp['w_router'][i], f32)
            wC["w_router"] = np.ascontiguousarray(wr.reshape(16, 128, 8).transpose(1, 0, 2).reshape(128, 128))
            wC["ident"] = ident
        ncC = _prog(('C', moe, final), lambda: build_phaseC(TOK, moe, final))
        mapsC = []
        for c in range(NCORE):
            m = dict(wC)
            m["xT"] = xT[c]
            m["oT"] = np.ascontiguousarray(np.stack([rB[hh]['oT'][:, c * TOK:(c + 1) * TOK] for hh in range(8)], axis=0))
            m["yT"] = np.ascontiguousarray(np.stack([rB[hh]['yT'][:, c * TOK:(c + 1) * TOK] for hh in range(8)], axis=0))
            mapsC.append(m)
        rC = _run(ncC, mapsC)
        xT = [np.asarray(rC[c]['outT'], f32) for c in range(NCORE)]
        del rB, rC, mapsC, wC
    out = np.concatenate([xT[c].reshape(2048, TOK).T for c in range(NCORE)], axis=0)
    return np.ascontiguousarray(out[None].astype(f32))
```
